# Optimizing a Trainium2 kernel written in Bass

```python
import math, functools
import jax, jax.numpy as jnp
from jax import lax
import numpy as np

D_MODEL = 1024
BATCH = 32
SEQ = 2048
DEPTH = 1

EPS = 1e-6
D_FF = 2816
MIX_WIDTH = D_MODEL
POOL_WIDTH = MIX_WIDTH // 2
N_POOL_GROUPS = 4
POOL_GROUP_DIM = POOL_WIDTH // N_POOL_GROUPS
POOL_WINDOWS = (2, 4, 8, 16)
ATTN_WIDTH = MIX_WIDTH // 2
N_HEADS = 8
HEAD_DIM = ATTN_WIDTH // N_HEADS
MOBA_BLOCK = 256
MOBA_TOPK = 3
Q_BLOCK = 128
N_BRANCHES = 2
IN_WIDTH = POOL_WIDTH + 3 * ATTN_WIDTH + N_BRANCHES * D_MODEL

kernel_name = "hybrid_pool_moba_gated_macaron"


def rms_norm(x, g):
    xf = x.astype(jnp.float32)
    y = xf * lax.rsqrt(jnp.mean(xf * xf, axis=-1, keepdims=True) + EPS)
    return (y * g.astype(jnp.float32)).astype(x.dtype)


def swiglu(x, w_gate, w_up, w_down):
    return (jax.nn.silu(x @ w_gate) * (x @ w_up)) @ w_down


def alibi_slopes(n_heads):
    return jnp.exp2(-8.0 * jnp.arange(1, n_heads + 1, dtype=jnp.float32) / n_heads)


def multiscale_pool(u, w_pool, pool_scale):
    B, S, _ = u.shape
    ug = u.reshape(B, S, N_POOL_GROUPS, POOL_GROUP_DIM)
    csum = jnp.cumsum(ug.astype(jnp.float32), axis=1)
    csum = jnp.pad(csum, ((0, 0), (1, 0), (0, 0), (0, 0)))
    t = jnp.arange(S)
    pooled = []
    for g, w in enumerate(POOL_WINDOWS):
        lo = jnp.maximum(t + 1 - w, 0)
        win_sum = csum[:, 1:, g] - csum[:, lo, g]
        count = (t + 1 - lo).astype(jnp.float32)
        pooled.append(win_sum / count[None, :, None])
    pooled = jnp.stack(pooled, axis=2)
    mixed = (pooled - ug.astype(jnp.float32)).astype(u.dtype)
    y = jnp.einsum('bsgc,gcd->bsgd', mixed, w_pool).reshape(B, S, POOL_WIDTH)
    return y * pool_scale


def moba_attention(q, k, v, slopes):
    B, S, H, Dh = q.shape
    BS = MOBA_BLOCK
    nb = -(-S // BS)
    pad = nb * BS - S
    qh = q.transpose(0, 2, 1, 3)
    kh = jnp.pad(k.transpose(0, 2, 1, 3), ((0, 0), (0, 0), (0, pad), (0, 0)))
    vh = jnp.pad(v.transpose(0, 2, 1, 3), ((0, 0), (0, 0), (0, pad), (0, 0)))
    kb = kh.reshape(B, H, nb, BS, Dh)
    vb = vh.reshape(B, H, nb, BS, Dh)

    t_all = jnp.arange(S)
    k_mean = jnp.mean(kb.astype(jnp.float32), axis=3)
    gate = jnp.einsum('bhsd,bhnd->bhsn', qh.astype(jnp.float32), k_mean)
    fully_past = jnp.arange(nb)[None, :] < (t_all // BS)[:, None]
    gate = jnp.where(fully_past[None, None], gate, -jnp.inf)
    topk = min(MOBA_TOPK, nb)
    _, sel_idx = lax.top_k(gate, topk)

    nqb = S // Q_BLOCK
    q_blocks = qh.reshape(B, H, nqb, Q_BLOCK, Dh).transpose(0, 2, 1, 3, 4).reshape(B * nqb, H, Q_BLOCK, Dh)
    idx_blocks = sel_idx.reshape(B, H, nqb, Q_BLOCK, topk).transpose(0, 2, 1, 3, 4).reshape(B * nqb, H, Q_BLOCK, topk)
    b_ids = jnp.repeat(jnp.arange(B), nqb)
    qb_ids = jnp.tile(jnp.arange(nqb), B)
    scale = Dh ** -0.5
    head_ix = jnp.arange(H)[:, None, None]
    pos_in_blk = jnp.arange(BS)

    def one_query_block(args):
        b, qi, qblk, idx = args
        kb_b = kb[b]
        vb_b = vb[b]
        tq = qi * Q_BLOCK + jnp.arange(Q_BLOCK)
        own = (qi * Q_BLOCK) // BS
        k_own = lax.dynamic_index_in_dim(kb_b, own, axis=1, keepdims=False)
        v_own = lax.dynamic_index_in_dim(vb_b, own, axis=1, keepdims=False)
        s_own = own * BS + pos_in_blk
        k_sel = kb_b[head_ix, idx]
        v_sel = vb_b[head_ix, idx]
        s_sel = idx[..., None] * BS + pos_in_blk
        slot_ok = jnp.arange(topk)[None, :] < (tq // BS)[:, None]

        l_sel = (jnp.einsum('hqd,hqjsd->hqjs', qblk, k_sel).astype(jnp.float32) * scale
                 - slopes[:, None, None, None] * (tq[None, :, None, None] - s_sel).astype(jnp.float32))
        l_sel = jnp.where(slot_ok[None, :, :, None], l_sel, -jnp.inf)
        l_own = (jnp.einsum('hqd,hsd->hqs', qblk, k_own).astype(jnp.float32) * scale
                 - slopes[:, None, None] * (tq[:, None] - s_own[None, :]).astype(jnp.float32))
        l_own = jnp.where((s_own[None, :] <= tq[:, None])[None], l_own, -jnp.inf)

        logits = jnp.concatenate([l_sel.reshape(H, Q_BLOCK, topk * BS), l_own], axis=-1)
        p = jax.nn.softmax(logits, axis=-1).astype(v.dtype)
        p_sel = p[..., :topk * BS].reshape(H, Q_BLOCK, topk, BS)
        p_own = p[..., topk * BS:]
        return (jnp.einsum('hqjs,hqjsd->hqd', p_sel, v_sel)
                + jnp.einsum('hqs,hsd->hqd', p_own, v_own))

    out = lax.map(one_query_block, (b_ids, qb_ids, q_blocks, idx_blocks))
    return out.reshape(B, nqb, H, Q_BLOCK, Dh).transpose(0, 1, 3, 2, 4).reshape(B, S, H * Dh)


def setup_inputs(seed: int = 0) -> dict:
    key = jax.random.key(seed)
    ks = jax.random.split(key, 17)
    L = DEPTH
    f32 = jnp.float32

    def w(k, shape, fan_in):
        return jax.random.normal(k, shape, f32) * fan_in ** -0.5

    def gain(k, shape):
        return 1.0 + 0.1 * jax.random.normal(k, shape, f32)

    return {
        "x": jax.random.normal(ks[0], (BATCH, SEQ, D_MODEL), f32),
        "ffn1_norm": gain(ks[1], (L, D_MODEL)),
        "ffn1_w_gate": w(ks[2], (L, D_MODEL, D_FF), D_MODEL),
        "ffn1_w_up": w(ks[3], (L, D_MODEL, D_FF), D_MODEL),
        "ffn1_w_down": w(ks[4], (L, D_FF, D_MODEL), D_FF),
        "mix_norm": gain(ks[5], (L, D_MODEL)),
        "w_in": w(ks[6], (L, D_MODEL, IN_WIDTH), D_MODEL),
        "pool_w": w(ks[7], (L, N_POOL_GROUPS, POOL_GROUP_DIM, POOL_GROUP_DIM), POOL_GROUP_DIM),
        "pool_scale": gain(ks[8], (L, POOL_WIDTH)),
        "w_branch_pool": w(ks[9], (L, POOL_WIDTH, D_MODEL), POOL_WIDTH),
        "w_branch_attn": w(ks[10], (L, ATTN_WIDTH, D_MODEL), ATTN_WIDTH),
        "w_out": w(ks[11], (L, D_MODEL, D_MODEL), D_MODEL),
        "ffn2_norm": gain(ks[12], (L, D_MODEL)),
        "ffn2_w_gate": w(ks[13], (L, D_MODEL, D_FF), D_MODEL),
        "ffn2_w_up": w(ks[14], (L, D_MODEL, D_FF), D_MODEL),
        "ffn2_w_down": w(ks[15], (L, D_FF, D_MODEL), D_FF),
        "final_norm": gain(ks[16], (D_MODEL,)),
    }


def reference(x, ffn1_norm, ffn1_w_gate, ffn1_w_up, ffn1_w_down, mix_norm, w_in,
              pool_w, pool_scale, w_branch_pool, w_branch_attn, w_out,
              ffn2_norm, ffn2_w_gate, ffn2_w_up, ffn2_w_down, final_norm):
    B, S, _ = x.shape
    slopes = alibi_slopes(N_HEADS)
    h = x
    for l in range(DEPTH):
        h = h + 0.5 * swiglu(rms_norm(h, ffn1_norm[l]), ffn1_w_gate[l], ffn1_w_up[l], ffn1_w_down[l])

        u = rms_norm(h, mix_norm[l])
        proj = u @ w_in[l]
        o1 = POOL_WIDTH
        o2 = o1 + ATTN_WIDTH
        o3 = o2 + ATTN_WIDTH
        o4 = o3 + ATTN_WIDTH
        u_pool = proj[..., :o1]
        q = proj[..., o1:o2].reshape(B, S, N_HEADS, HEAD_DIM)
        k = proj[..., o2:o3].reshape(B, S, N_HEADS, HEAD_DIM)
        v = proj[..., o3:o4].reshape(B, S, N_HEADS, HEAD_DIM)
        gates = jax.nn.sigmoid(proj[..., o4:].astype(jnp.float32)).astype(h.dtype)
        gates = gates.reshape(B, S, N_BRANCHES, D_MODEL)

        y_pool = multiscale_pool(u_pool, pool_w[l], pool_scale[l]) @ w_branch_pool[l]
        y_attn = moba_attention(q, k, v, slopes) @ w_branch_attn[l]
        merged = gates[:, :, 0] * y_pool + gates[:, :, 1] * y_attn
        h = h + merged @ w_out[l]

        h = h + 0.5 * swiglu(rms_norm(h, ffn2_norm[l]), ffn2_w_gate[l], ffn2_w_up[l], ffn2_w_down[l])
    return rms_norm(h, final_norm)
```

```python
import contextlib
import numpy as np
import concourse.bass as bass
import concourse.mybir as mybir
from concourse.bass_utils import run_bass_kernel_spmd

F32 = mybir.dt.float32
BF16 = mybir.dt.bfloat16
AF = mybir.ActivationFunctionType
ALU = mybir.AluOpType
AX = mybir.AxisListType

NCORES = 8
NSEQ = 4
S = 2048
D = 1024
DFF = 2816
NCH = DFF // 128
NT = S // 128
NTC = S // 512
EPS = 1e-6
MBIG = -240000.0
FILL = False
ENGS = ("pe", "act", "dve", "pool", "sp")
XNAMES = {"hT", "wd", "QA", "KA", "V", "PT", "OT", "zT", "gat", "pl", "mrg", "wout", "obuf"}


class Op:
    __slots__ = ("eng", "fn", "reads", "writes", "is_dma", "sem_key", "waits",
                 "signal", "need_signal", "idx", "is_out")

    def __init__(self, eng, fn, reads, writes, is_dma=False, sem_key=None, is_out=False):
        self.eng = eng
        self.fn = fn
        self.reads = tuple(reads)
        self.writes = tuple(writes)
        self.is_dma = is_dma
        self.sem_key = sem_key
        self.waits = []
        self.signal = None
        self.need_signal = False
        self.is_out = is_out


def _isx(k):
    return isinstance(k, tuple) and k[0] in XNAMES


class Prog:
    def __init__(self):
        self.ops = []
        self.final_waits = {}

    def _fix(self, reads, writes):
        reads = list(reads)
        if any(_isx(k) for k in reads) or any(_isx(k) for k in writes):
            reads.append("Xep")
        return reads, list(writes)

    def add(self, eng, fn, reads=(), writes=()):
        reads, writes = self._fix(reads, writes)
        op = Op(eng, fn, reads, writes)
        self.ops.append(op)
        return op

    def dma(self, queue, fn, reads=(), writes=(), sem_key=None, is_out=False):
        reads, writes = self._fix(reads, writes)
        op = Op(queue, fn, reads, writes, is_dma=True, sem_key=sem_key, is_out=is_out)
        self.ops.append(op)
        return op

    def resolve(self):
        last_writer = {}
        readers = {}
        deps_of = []
        for i, op in enumerate(self.ops):
            op.idx = i
            deps = set()
            for k in op.reads:
                w = last_writer.get(k)
                if w is not None:
                    deps.add(w)
            for k in op.writes:
                w = last_writer.get(k)
                if w is not None:
                    deps.add(w)
                for r in readers.get(k, ()):
                    deps.add(r)
            for k in op.reads:
                readers.setdefault(k, []).append(i)
            for k in op.writes:
                last_writer[k] = i
                readers[k] = []
            deps.discard(i)
            real = []
            for d in deps:
                dop = self.ops[d]
                if dop.is_dma:
                    real.append(d)
                    continue
                if dop.eng == op.eng:
                    if op.is_dma:
                        real.append(d)
                        continue
                    if op.eng == "pe":
                        continue
                    if any(k in dop.writes for k in op.reads):
                        real.append(d)
                    continue
                real.append(d)
            deps_of.append(real)
            for d in real:
                self.ops[d].need_signal = True
        for op in self.ops:
            if op.is_dma and op.is_out:
                op.need_signal = True
        counters = {}
        for op in self.ops:
            if op.is_dma:
                sn = ("dma", op.sem_key)
                counters[sn] = counters.get(sn, 0) + 16
                op.signal = (sn, counters[sn])
            elif op.need_signal:
                sn = ("eng", op.eng)
                counters[sn] = counters.get(sn, 0) + 1
                op.signal = (sn, counters[sn])
        self.counters = counters
        waited = {e: {} for e in ENGS}
        for i, op in enumerate(self.ops):
            need = {}
            for d in deps_of[i]:
                sn, v = self.ops[d].signal
                if v > need.get(sn, 0):
                    need[sn] = v
            for sn, v in need.items():
                if waited[op.eng].get(sn, 0) >= v:
                    continue
                waited[op.eng][sn] = v
                op.waits.append((sn, v))
        for op in self.ops:
            if op.is_dma and op.is_out:
                sn, v = op.signal
                cur = self.final_waits.setdefault(op.eng, {})
                if v > cur.get(sn, 0):
                    cur[sn] = v
        return counters

    def sem_names(self):
        return sorted(self.counters.keys(), key=str)

    def emit(self, nc, sems):
        by_eng = {e: [] for e in ENGS}
        for op in self.ops:
            by_eng[op.eng].append(op)

        def run(engobj, ename):
            for op in by_eng[ename]:
                for sn, v in op.waits:
                    engobj.wait_ge(sems[sn], v)
                ins = op.fn(engobj)
                if op.signal is not None:
                    sn, v = op.signal
                    if op.is_dma:
                        ins.then_inc(sems[sn], 16)
                    elif op.need_signal:
                        ins.then_inc(sems[sn], 1)
            for sn, v in self.final_waits.get(ename, {}).items():
                engobj.wait_ge(sems[sn], v)

        with nc.Block() as block:
            @block.tensor
            def _(e):
                run(e, "pe")

            @block.scalar
            def _(e):
                run(e, "act")

            @block.vector
            def _(e):
                run(e, "dve")

            @block.gpsimd
            def _(e):
                run(e, "pool")

            @block.sync
            def _(e):
                run(e, "sp")


def build_program(nseq=NSEQ, debug=False):
    nc = bass.Bass("TRN2", target_bir_lowering=False)
    dbg_d = nc.dram_tensor("dbg", [4, S, D], F32, kind="ExternalOutput").ap() if debug else None
    dbg_ot = nc.dram_tensor("dbg_ot", [128, 4, S], F32, kind="ExternalOutput").ap() if debug else None
    dbg_zt = nc.dram_tensor("dbg_zt", [128, 4, S], F32, kind="ExternalOutput").ap() if debug else None
    dbg_mrg = nc.dram_tensor("dbg_mrg", [128, 8, S], F32, kind="ExternalOutput").ap() if debug else None

    def din(name, shape):
        return nc.dram_tensor(name, list(shape), F32, kind="ExternalInput").ap()

    x_d = din("x", [nseq, S, D])
    out_d = nc.dram_tensor("out", [nseq, S, D], F32, kind="ExternalOutput").ap()
    wgt = {
        "w1g": din("w1g", [NCH, 128, 8, 128]), "w1u": din("w1u", [NCH, 128, 8, 128]),
        "w2g": din("w2g", [NCH, 128, 8, 128]), "w2u": din("w2u", [NCH, 128, 8, 128]),
        "win": din("win", [32, 128, 8, 128]),
        "wbp": din("wbp", [8, 128, 4, 128]), "wba": din("wba", [8, 128, 4, 128]),
    }
    w1d_d = din("w1d", [DFF, D])
    w2d_d = din("w2d", [DFF, D])
    wout_d = din("wout", [D, D])
    poolw_d = din("poolw", [128, 4, 128])
    gains_d = din("gains", [4, 128, D])
    pscale_d = din("pscale", [128, 4])
    ident_d = din("ident", [128, 128])
    tri_d = din("tri", [128, 128])
    alibi_d = din("alibiq", [8, 2, S])
    alibik_d = din("alibik", [8, 2, S])
    kaaug_d = din("kaaug", [10, S])
    expb_d = din("expb", [128, 8, 16])
    invc_d = din("invc", [128, 4, 16])

    h = nc.alloc_sbuf_tensor("h", [128, NT, D], F32)
    uT = nc.alloc_sbuf_tensor("uT", [128, 8, S], BF16)
    ring = nc.alloc_sbuf_tensor("ring", [128, 8, 8, 128], BF16)
    gain = nc.alloc_sbuf_tensor("gain", [128, D], F32)
    ub = nc.alloc_sbuf_tensor("ub", [128, 2, D], BF16)
    junk = nc.alloc_sbuf_tensor("junk", [128, D], BF16)
    tmp = nc.alloc_sbuf_tensor("tmp", [128, 4, 512], F32)
    ss = nc.alloc_sbuf_tensor("ss", [128, NT], F32)
    rstd = nc.alloc_sbuf_tensor("rstd", [128, NT], F32)
    ident = nc.alloc_sbuf_tensor("ident_s", [128, 128], BF16)
    tri = nc.alloc_sbuf_tensor("tri_s", [128, 128], BF16)
    expb = nc.alloc_sbuf_tensor("expb_s", [128, 8, 16], F32)
    invc = nc.alloc_sbuf_tensor("invc_s", [128, 4, 16], F32)
    pscale = nc.alloc_sbuf_tensor("pscale_s", [128, 4], F32)
    poolw = nc.alloc_sbuf_tensor("poolw_s", [128, 4, 128], BF16)
    fscr = nc.alloc_sbuf_tensor("fscr", [128, 8], F32)
    XR = 32768
    XB = nc.alloc_sbuf_tensor("X", [128, XR], BF16)
    XF = XB.bitcast(F32)
    XRF = XR // 2
    ps = nc.alloc_psum_tensor("ps", [128, 4096], F32)
    psb = ps.bitcast(BF16)
    rec = nc.alloc_sbuf_tensor("rec", [128, 512], F32)

    def xb(off, dims, p0=0, np_=128):
        return bass.AP(XB, p0 * XR + off, [[XR, np_]] + [list(d) for d in dims])

    def xf(off, dims, p0=0, np_=128):
        return bass.AP(XF, p0 * XRF + off, [[XRF, np_]] + [list(d) for d in dims])

    def bk(i, c0, n, p0=0, np_=128):
        return bass.AP(ps, p0 * 4096 + i * 512 + c0, [[4096, np_], [1, n]])

    HT = lambda b: b * 8192
    WD = lambda b: 16384 + b * 4096
    QA = lambda e: e * 2048
    KA = lambda e: 4096 + e * 2048
    VO = 8192
    PT = lambda i: 11264 + i * 1024
    GCP_F = 7168
    CMP_F = 7296
    RANK_F = 7424
    MBP = 14880
    KMS_F = 8016
    KMB = 16064
    OTO = 16384
    ZTO = 24576
    PB_F, TA_F, TB_F = 0, 2064, 4128
    MIXB = 12384
    MRG = 0
    WOUT = 16384
    OBUF_F = 0

    P = Prog()
    att_state = {"sp": 0}
    pools = {"main": [0, 1, 2, 3, 4, 5], "acc": [6, 7], "all": [0, 1, 2, 3, 4, 5, 6, 7], "aux": ([4] if FILL else [4, 5])}
    pool_ctr = {"main": 0, "acc": 0, "all": 0, "aux": 0}

    def nbank(pool="main"):
        lst = pools[pool]
        b = lst[pool_ctr[pool] % len(lst)]
        pool_ctr[pool] += 1
        return b

    def fence():
        P.add("pool", lambda e: e.memset(fscr[:, 0:1], 0.0), reads=[], writes=["Xep"])

    uses = []
    for s in range(nseq):
        for c in range(NCH):
            uses += [("w1g", c), ("w1u", c)]
        for j in range(4):
            uses += [("win", 8 + j), ("win", 12 + j), ("win", 4 + j)]
        for g in range(4):
            uses += [("win", g)]
        for c in range(8):
            uses += [("win", 16 + c), ("win", 24 + c), ("bpba", c)]
        for c in range(NCH):
            uses += [("w2g", c), ("w2u", c)]
    ring_state = {"emitted": 0, "used": 0}
    LOOKAHEAD = 5

    def ring_emit_upto(n):
        while ring_state["emitted"] < min(n, len(uses)):
            m = ring_state["emitted"]
            kind, idx = uses[m]
            slot = m % 8
            if kind == "bpba":
                P.dma("pool", lambda e, slot=slot, idx=idx: e.dma_start(out=ring[:, slot, 0:4, :], in_=wgt["wbp"][idx]),
                      writes=[("ring", slot)], sem_key=("ring", slot, 0))
                P.dma("pool", lambda e, slot=slot, idx=idx: e.dma_start(out=ring[:, slot, 4:8, :], in_=wgt["wba"][idx]),
                      writes=[("ringb", slot)], reads=[("ring", slot)], sem_key=("ring", slot, 1))
            else:
                P.dma("pool", lambda e, slot=slot, kind=kind, idx=idx: e.dma_start(out=ring[:, slot, :, :], in_=wgt[kind][idx]),
                      writes=[("ring", slot), ("ringb", slot)], sem_key=("ring", slot, 0))
            ring_state["emitted"] += 1

    def ring_use(kind, idx):
        m = ring_state["used"]
        assert uses[m] == (kind, idx), (uses[m], kind, idx)
        ring_emit_upto(m + 1 + LOOKAHEAD)
        ring_state["used"] += 1
        return m % 8

    def rk(slot):
        return [("ring", slot), ("ringb", slot)]

    P.dma("pool", lambda e: e.dma_start(out=ident[:], in_=ident_d), writes=["ident"], sem_key="c_ident")
    P.dma("pool", lambda e: e.dma_start(out=tri[:], in_=tri_d), writes=["tri"], sem_key="c_tri")
    P.dma("pool", lambda e: e.dma_start(out=poolw[:], in_=poolw_d), writes=["poolw"], sem_key="c_poolw")
    P.dma("sp", lambda e: e.dma_start(out=expb[:], in_=expb_d), writes=["expb"], sem_key="c_expb")
    P.dma("sp", lambda e: e.dma_start(out=invc[:], in_=invc_d), writes=["invc"], sem_key="c_invc")
    P.dma("sp", lambda e: e.dma_start(out=pscale[:], in_=pscale_d), writes=["pscale"], sem_key="c_pscale")

    def load_x(s):
        for t in range(NT):
            P.dma("sp", lambda e, t=t: e.dma_start(out=h[:, t, :], in_=x_d[s, t * 128:(t + 1) * 128, :]),
                  writes=[("h", t, 0), ("h", t, 1)], sem_key=("x", t))

    def norm_stats(gidx):
        P.dma("sp", lambda e: e.dma_start(out=gain[:], in_=gains_d[gidx]), writes=["gain"], sem_key="gain")
        P.add("act", lambda e: e.memzero(ss[:]), writes=[("ss", t) for t in range(NT)])
        for t in range(NT):
            P.add("act", lambda e, t=t: e.activation(junk[:], h[:, t, :], AF.Square, accum_out=ss[:, t:t + 1]),
                  reads=[("h", t, 0), ("h", t, 1), ("ss", t)], writes=["junk", ("ss", t)])
        allss = [("ss", t) for t in range(NT)]
        P.add("dve", lambda e: e.tensor_scalar(rstd[:], ss[:], 1.0 / D, EPS, ALU.mult, ALU.add),
              reads=allss, writes=["rstd"])
        P.add("act", lambda e: e.activation(rstd[:], rstd[:], AF.Sqrt), reads=["rstd"], writes=["rstd"])
        P.add("dve", lambda e: e.reciprocal(rstd[:], rstd[:]), reads=["rstd"], writes=["rstd"])

    def norm_to_uT(gidx):
        norm_stats(gidx)

        def emit_tiles(t0):
            for t in range(t0, t0 + 4):
                b = t % 2
                P.add("dve", lambda e, t=t, b=b: e.scalar_tensor_tensor(ub[:, b, :], h[:, t, :], rstd[:, t:t + 1], gain[:],
                                                                        ALU.mult, ALU.mult),
                      reads=[("h", t, 0), ("h", t, 1), "rstd", "gain"], writes=[("ub", b)])
                bi = nbank("all")

                def tr(e, b=b, bi=bi):
                    ins = None
                    for k in range(8):
                        ins = e.transpose(bass.AP(psb, bi * 1024 + k * 128, [[8192, 128], [1, 128]]),
                                          ub[:, b, k * 128:(k + 1) * 128], ident[:])
                    return ins
                P.add("pe", tr, reads=[("ub", b), "ident"], writes=[("bank", bi)])
                P.add("act", lambda e, t=t, bi=bi: e.activation(
                    bass.AP(uT, t * 128, [[8 * S, 128], [S, 8], [1, 128]]),
                    bass.AP(psb, bi * 1024, [[8192, 128], [128, 8], [1, 128]]), AF.Copy),
                    reads=[("bank", bi)], writes=[("uT", t)])
        return [lambda t0=t0: emit_tiles(t0) for t0 in (0, 4, 8, 12)]

    def ffn(gk, uk, wd_d, pending):
        parts = [(0, 4), (4, 8), (8, 12), (12, 16), (16, 20), (20, 22)]
        fence()

        def gu(pi):
            c0, c1 = parts[pi]
            hb = pi % 2
            P.dma("pool", lambda e, c0=c0, c1=c1, hb=hb: e.dma_start(
                out=xb(WD(hb), [[1024, c1 - c0], [1, 1024]]),
                in_=wd_d[c0 * 128:c1 * 128, :].rearrange("(c p) n -> p c n", p=128)),
                writes=[("wd", hb)], sem_key=("wd", hb))
            for c in range(c0, c1):
                sg = ring_use(gk, c)
                su = ring_use(uk, c)
                for tc in range(NTC):
                    if pending:
                        pending.pop(0)()
                    bg = nbank("all")
                    bu = nbank("all")

                    def mm(e, sg=sg, su=su, tc=tc, bg=bg, bu=bu):
                        ins = None
                        for k in range(8):
                            ins = e.matmul(bk(bg, 0, 512), ring[:, sg, k, :], uT[:, k, tc * 512:(tc + 1) * 512],
                                           start=(k == 0), stop=(k == 7))
                        for k in range(8):
                            ins = e.matmul(bk(bu, 0, 512), ring[:, su, k, :], uT[:, k, tc * 512:(tc + 1) * 512],
                                           start=(k == 0), stop=(k == 7))
                        return ins
                    P.add("pe", mm, reads=rk(sg) + rk(su) + [("uT", 4 * tc + i) for i in range(4)],
                          writes=[("bank", bg), ("bank", bu)])
                    tb_ = (c * NTC + tc) % 4
                    P.add("act", lambda e, bg=bg, tb_=tb_: e.activation(tmp[:, tb_, :], bk(bg, 0, 512), AF.Silu),
                          reads=[("bank", bg)], writes=[("tmp", tb_)])
                    P.add("dve", lambda e, bu=bu, tb_=tb_, hb=hb, cc=c - c0, tc=tc: e.tensor_tensor(
                        xb(HT(hb) + cc * 2048 + tc * 512, [[1, 512]]), tmp[:, tb_, :], bk(bu, 0, 512), ALU.mult),
                        reads=[("tmp", tb_), ("bank", bu)], writes=[("hT", hb, c - c0, tc)])

        def down(pi):
            c0, c1 = parts[pi]
            hb = pi % 2
            n = c1 - c0
            for t in range(NT):
                for hf in range(2):
                    bi = nbank("all")

                    def mm(e, t=t, hf=hf, bi=bi, n=n, hb=hb):
                        ins = None
                        for cc in range(n):
                            ins = e.matmul(bk(bi, 0, 512), xb(HT(hb) + cc * 2048 + t * 128, [[1, 128]]),
                                           xb(WD(hb) + cc * 1024 + hf * 512, [[1, 512]]),
                                           start=(cc == 0), stop=(cc == n - 1))
                        return ins
                    P.add("pe", mm, reads=[("hT", hb, cc, t // 4) for cc in range(n)] + [("wd", hb)],
                          writes=[("bank", bi)])
                    P.add("dve", lambda e, t=t, hf=hf, bi=bi: e.scalar_tensor_tensor(
                        h[:, t, hf * 512:(hf + 1) * 512], bk(bi, 0, 512), 0.5, h[:, t, hf * 512:(hf + 1) * 512],
                        ALU.mult, ALU.add),
                        reads=[("bank", bi), ("h", t, hf)], writes=[("h", t, hf)])

        gu(0)
        for pi in range(1, len(parts)):
            gu(pi)
            down(pi - 1)
        down(len(parts) - 1)

    def attention(pending):
        fence()
        for e_ in range(2):
            P.dma("pool", lambda e, e_=e_: e.dma_start(out=xb(KA(e_), [[1, S]], p0=64, np_=10), in_=kaaug_d),
                  writes=[("KA", e_, "aug")], sem_key=("kaaug", e_))
            P.dma("pool", lambda e, e_=e_: e.dma_start(out=xb(QA(e_), [[1, S]], p0=74, np_=2), in_=kaaug_d[8:10, :]),
                  writes=[("QA", e_, "ones")], sem_key=("qaones", e_))
        P.add("pool", lambda e: e.memset(xb(MBP, [[1, 8 * 2 * 72]]), 0.0), writes=[("gat", "mbp")])
        P.add("pool", lambda e: e.memset(xb(VO, [[1, 16 * 192]]), 1.0), writes=[("V", "all")])
        for j in range(4):
            sk = ring_use("win", 8 + j)
            sv = ring_use("win", 12 + j)
            sq = ring_use("win", 4 + j)
            for e_ in range(2):
                hh = 2 * j + e_
                P.add("pool", lambda e, e_=e_: e.memset(xb(QA(e_), [[1, S]], p0=64, np_=8), 0.0),
                      writes=[("QA", e_, "mask")])
                P.dma("pool", lambda e, e_=e_, hh=hh: e.dma_start(out=xb(QA(e_), [[1, S]], p0=72, np_=2), in_=alibi_d[hh]),
                      writes=[("QA", e_, "alibi")], sem_key=("alibi", e_))
                P.dma("pool", lambda e, e_=e_, hh=hh: e.dma_start(out=xb(KA(e_), [[1, S]], p0=74, np_=2), in_=alibik_d[hh]),
                      writes=[("KA", e_, "kpos")], sem_key=("alibik", e_))
            for tc in range(NTC):
                if pending:
                    pending.pop(0)()
                bi = nbank("aux")

                def mmk(e, sk=sk, tc=tc, bi=bi):
                    ins = None
                    for k in range(8):
                        ins = e.matmul(bk(bi, 0, 512), ring[:, sk, k, :], uT[:, k, tc * 512:(tc + 1) * 512],
                                       start=(k == 0), stop=(k == 7))
                    return ins
                P.add("pe", mmk, reads=rk(sk) + [("uT", 4 * tc + i) for i in range(4)], writes=[("bank", bi)])
                for e_ in range(2):
                    P.add("dve", lambda e, tc=tc, bi=bi, e_=e_: e.tensor_copy(
                        xb(KA(e_) + tc * 512, [[1, 512]], np_=64), bk(bi, 0, 512, p0=64 * e_, np_=64)),
                        reads=[("bank", bi)], writes=[("KA", e_, tc)])
                    P.add("dve", lambda e, tc=tc, bi=bi, e_=e_: e.tensor_reduce(
                        xf(KMS_F + e_ * 8 + 2 * tc, [[1, 2]], np_=64),
                        bass.AP(ps, e_ * 64 * 4096 + bi * 512, [[4096, 64], [256, 2], [1, 256]]), AX.X, ALU.add),
                        reads=[("bank", bi)], writes=[("gat", "kms", e_, tc)])
            P.add("dve", lambda e: e.tensor_scalar(xb(KMB, [[1, 16]], np_=64), xf(KMS_F, [[1, 16]], np_=64),
                                                   1.0 / 256, 0.0, ALU.mult, ALU.add),
                  reads=[("gat", "kms", e_, tc) for e_ in range(2) for tc in range(NTC)], writes=[("gat", "kmb")])
            for t4 in range(4):
                bi = nbank("aux")

                def mmv(e, sv=sv, t4=t4, bi=bi):
                    ins = None
                    for tt in range(4):
                        t = t4 * 4 + tt
                        for k in range(8):
                            ins = e.matmul(bk(bi, tt * 128, 128), uT[:, k, t * 128:(t + 1) * 128], ring[:, sv, k, :],
                                           start=(k == 0), stop=(k == 7))
                    return ins
                P.add("pe", mmv, reads=rk(sv) + [("uT", t4 * 4 + i) for i in range(4)], writes=[("bank", bi)])
                P.add("dve", lambda e, t4=t4, bi=bi: e.tensor_copy(
                    xb(VO + t4 * 4 * 192, [[192, 4], [128, 2], [1, 64]]),
                    bass.AP(ps, bi * 512, [[4096, 128], [128, 4], [64, 2], [1, 64]])),
                    reads=[("bank", bi), ("V", "all")], writes=[("V", t4)])
            for tc in range(NTC):
                bi = nbank("aux")

                def mmq(e, sq=sq, tc=tc, bi=bi):
                    ins = None
                    for k in range(8):
                        ins = e.matmul(bk(bi, 0, 512), ring[:, sq, k, :], uT[:, k, tc * 512:(tc + 1) * 512],
                                       start=(k == 0), stop=(k == 7))
                    return ins
                P.add("pe", mmq, reads=rk(sq) + [("uT", 4 * tc + i) for i in range(4)], writes=[("bank", bi)])
                for e_ in range(2):
                    P.add("dve", lambda e, tc=tc, bi=bi, e_=e_: e.tensor_copy(
                        xb(QA(e_) + tc * 512, [[1, 512]], np_=64), bk(bi, 0, 512, p0=64 * e_, np_=64)),
                        reads=[("bank", bi)], writes=[("QA", e_, tc)])
            bg = nbank("aux")

            def mmg(e, bg=bg):
                ins = None
                for t8 in range(8):
                    t = 8 + t8
                    for e_ in range(2):
                        ins = e.matmul(bk(bg, (t8 * 2 + e_) * 8, 8), xb(QA(e_) + t * 128, [[1, 128]], np_=64),
                                       xb(KMB + e_ * 8, [[1, 8]], np_=64), start=True, stop=True)
                return ins
            P.add("pe", mmg, reads=[("QA", e_, tc) for e_ in range(2) for tc in (2, 3)] + [("gat", "kmb")],
                  writes=[("bank", bg)])
            P.add("dve", lambda e, bg=bg: e.tensor_copy(xf(GCP_F, [[1, 128]]), bk(bg, 0, 128)),
                  reads=[("bank", bg)], writes=[("gat", "gcp")])
            for t8 in range(8):
                n = (8 + t8) // 2
                gj = xf(GCP_F + t8 * 16, [[8, 2], [0, n], [1, n]])
                gi = xf(GCP_F + t8 * 16, [[8, 2], [1, n], [0, n]])
                P.add("dve", lambda e, gj=gj, gi=gi, n=n: e.tensor_tensor(xf(CMP_F, [[n * n, 2], [n, n], [1, n]]), gj, gi, ALU.is_gt),
                      reads=[("gat", "gcp")], writes=[("gat", "cmp")])
                P.add("dve", lambda e, n=n: e.tensor_reduce(xf(RANK_F, [[8, 2], [1, n]]),
                                                            xf(CMP_F, [[n * n, 2], [n, n], [1, n]]), AX.X, ALU.add),
                      reads=[("gat", "cmp")], writes=[("gat", "rank")])
                P.add("dve", lambda e, n=n, t8=t8: e.tensor_scalar(
                    xb(MBP + t8 * 144 + 64, [[72, 2], [1, n]]), xf(RANK_F, [[8, 2], [1, n]]), 2.5, MBIG, ALU.is_ge, ALU.mult),
                    reads=[("gat", "rank"), ("gat", "mbp")], writes=[("gat", "mb", t8)])
            for e_ in range(2):
                for hf in range(2):
                    bi = nbank("aux")

                    def mmt(e, e_=e_, hf=hf, bi=bi):
                        ins = None
                        for tt in range(4):
                            t8 = hf * 4 + tt
                            ins = e.matmul(bk(bi, tt * 128, 128, np_=72), xb(MBP + t8 * 144 + e_ * 72, [[1, 72]]),
                                           ident[:], start=True, stop=True)
                        return ins
                    P.add("pe", mmt, reads=[("gat", "mb", hf * 4 + tt) for tt in range(4)] + ["ident"],
                          writes=[("bank", bi)])
                    P.add("dve", lambda e, e_=e_, hf=hf, bi=bi: e.tensor_copy(
                        xb(QA(e_) + 1024 + hf * 512, [[1, 512]], p0=64, np_=8), bk(bi, 0, 512, p0=64, np_=8)),
                        reads=[("bank", bi), ("QA", e_, "mask")], writes=[("QA", e_, "mask2", hf)])
            steps = []
            for e_ in range(2):
                for qc in range(NTC):
                    bo = nbank("acc")
                    nkt = 4 * qc + 4
                    for p in range(nkt // 2):
                        steps.append(dict(e_=e_, qc=qc, p=p, bo=bo, nkt=nkt, jj=j, last=(p == nkt // 2 - 1)))

            def s_step(st):
                e_, qc, p = st["e_"], st["qc"], st["p"]
                qa_keys = [("QA", e_, "mask"), ("QA", e_, "alibi"), ("QA", e_, "ones"),
                           ("QA", e_, "mask2", 0), ("QA", e_, "mask2", 1)]
                ka_keys = [("KA", e_, "aug"), ("KA", e_, "kpos")]
                sp = att_state["sp"] % 2
                pp = att_state["sp"] % 3
                att_state["sp"] += 1
                kt0 = 2 * p
                r0 = kt0 - 4 * qc
                c0 = 128 * r0 if r0 > 0 else 0

                def mms(e, c0=c0, sp=sp, kt0=kt0, e_=e_, qc=qc):
                    ins = None
                    for i in range(2):
                        kt = kt0 + i
                        r = kt - 4 * qc
                        b = 2 * sp + i
                        ins = e.matmul(bk(b, c0, 512 - c0), xb(KA(e_) + kt * 128, [[1, 128]], np_=76),
                                       xb(QA(e_) + qc * 512 + c0, [[1, 512 - c0]], np_=76),
                                       start=True, stop=(r < 0))
                        if r >= 0:
                            ins = e.matmul(bk(b, 128 * r, 128), ident[:], tri[:], start=False, stop=True)
                    return ins
                P.add("pe", mms, reads=[("KA", e_, kt0 // 4), ("QA", e_, qc), "ident", "tri"] + qa_keys + ka_keys,
                      writes=[("bank", 2 * sp), ("bank", 2 * sp + 1)])
                P.add("act", lambda e, c0=c0, sp=sp, pp=pp: e.activation(
                    xb(PT(pp) + c0, [[512, 2], [1, 512 - c0]]),
                    bass.AP(ps, 2 * sp * 512 + c0, [[4096, 128], [512, 2], [1, 512 - c0]]), AF.Exp, scale=0.125),
                    reads=[("bank", 2 * sp), ("bank", 2 * sp + 1)], writes=[("PT", pp)])
                return (st, kt0, pp)

            def pv_step(st, kt0, pp):
                e_, qc, bo, nkt, jj = st["e_"], st["qc"], st["bo"], st["nkt"], st["jj"]

                def mmpv(e):
                    ins = None
                    for i in range(2):
                        kt = kt0 + i
                        r = kt - 4 * qc
                        c0i = 128 * r if r > 0 else 0
                        ins = e.matmul(bk(bo, c0i, 512 - c0i), xb(VO + kt * 192 + e_ * 64, [[1, 128]]),
                                       xb(PT(pp) + i * 512 + c0i, [[1, 512 - c0i]]),
                                       start=(kt == 0), stop=(kt == nkt - 1))
                    return ins
                P.add("pe", mmpv, reads=[("PT", pp), ("V", kt0 // 4), ("V", "all")], writes=[("bank", bo)])
                if FILL:
                    P.add("pe", lambda e: e.matmul(bk(5, 0, 384), ident[:], uT[:, 0, 0:384], start=True, stop=True),
                          reads=[], writes=[])
                if st["last"]:
                    if e_ == 0:
                        P.add("dve", lambda e: e.reciprocal(rec[0:64, :], bk(bo, 0, 512, p0=64, np_=64)),
                              reads=[("bank", bo)], writes=["rec"])
                        P.add("dve", lambda e: e.tensor_tensor(
                            xb(OTO + jj * 2048 + qc * 512, [[1, 512]], np_=64), bk(bo, 0, 512, np_=64),
                            rec[0:64, :], ALU.mult),
                            reads=[("bank", bo), "rec"], writes=[("OT", jj, 0, qc)])
                    else:
                        P.add("dve", lambda e: e.reciprocal(rec[64:128, :], bk(bo, 0, 512, np_=64)),
                              reads=[("bank", bo)], writes=["rec"])
                        P.add("dve", lambda e: e.tensor_tensor(
                            xb(OTO + jj * 2048 + qc * 512, [[1, 512]], p0=64, np_=64), bk(bo, 0, 512, p0=64, np_=64),
                            rec[64:128, :], ALU.mult),
                            reads=[("bank", bo), "rec"], writes=[("OT", jj, 1, qc)])

            prev = None
            for st in steps:
                cur = s_step(st)
                if prev is not None:
                    pv_step(*prev)
                prev = cur
            pv_step(*prev)

    def pool_mixer():
        P.add("pool", lambda e: e.memset(fscr[:, 1:2], 0.0), reads=[("OT", j, e_, qc) for j in range(4) for e_ in range(2) for qc in range(NTC)],
              writes=["Xep"])
        for off in (PB_F, TA_F, TB_F):
            P.add("pool", lambda e, off=off: e.memset(xf(off, [[1, 16]]), 0.0), writes=[("pl", "pad", off)])
        for g in range(4):
            sp_ = ring_use("win", g)
            w = 2 ** (g + 1)
            for tc in range(NTC):
                bi = nbank()

                def mmp(e, sp_=sp_, tc=tc, bi=bi):
                    ins = None
                    for k in range(8):
                        ins = e.matmul(bk(bi, 0, 512), ring[:, sp_, k, :], uT[:, k, tc * 512:(tc + 1) * 512],
                                       start=(k == 0), stop=(k == 7))
                    return ins
                P.add("pe", mmp, reads=rk(sp_) + [("uT", 4 * tc + i) for i in range(4)], writes=[("bank", bi)])
                P.add("act", lambda e, tc=tc, bi=bi: e.activation(xf(PB_F + 16 + tc * 512, [[1, 512]]), bk(bi, 0, 512), AF.Copy),
                      reads=[("bank", bi), ("pl", "pad", PB_F)], writes=[("pl", "p", tc)])
            pkeys = [("pl", "p", tc) for tc in range(NTC)]
            src = PB_F
            dsts = [TA_F, TB_F]
            for i in range(g + 1):
                sh = 2 ** i
                dst = dsts[i % 2]
                P.add("pool", lambda e, src=src, dst=dst, sh=sh: e.tensor_tensor(
                    xf(dst + 16, [[1, S]]), xf(src + 16, [[1, S]]), xf(src + 16 - sh, [[1, S]]), ALU.add),
                    reads=pkeys + [("pl", "sum", src), ("pl", "pad", src)], writes=[("pl", "sum", dst), ("pl", "pad", dst)])
                src = dst
            P.add("dve", lambda e, src=src, w=w: e.scalar_tensor_tensor(
                xb(MIXB, [[1, S]]), xf(src + 16, [[1, S]]), 1.0 / w, xf(PB_F + 16, [[1, S]]), ALU.mult, ALU.subtract),
                reads=pkeys + [("pl", "sum", src)], writes=[("pl", "mix")])
            P.add("pool", lambda e, src=src, g=g: e.tensor_tensor(
                xf(TA_F if src == TB_F else TB_F, [[1, 16]]), xf(src + 16, [[1, 16]]), invc[:, g, :], ALU.mult),
                reads=[("pl", "sum", src), "invc"], writes=[("pl", "pad", TA_F if src == TB_F else TB_F), ("pl", "fix")])
            P.add("pool", lambda e, src=src: e.tensor_tensor(
                xb(MIXB, [[1, 16]]), xf(TA_F if src == TB_F else TB_F, [[1, 16]]), xf(PB_F + 16, [[1, 16]]), ALU.subtract),
                reads=[("pl", "fix"), ("pl", "mix")] + pkeys, writes=[("pl", "mix2")])
            P.add("pool", lambda e, src=src: e.memset(xf(TA_F if src == TB_F else TB_F, [[1, 16]]), 0.0),
                  reads=[("pl", "mix2")], writes=[("pl", "pad", TA_F if src == TB_F else TB_F), ("pl", "fix")])
            for tc in range(NTC):
                bi = nbank()
                P.add("pe", lambda e, g=g, tc=tc, bi=bi: e.matmul(bk(bi, 0, 512), poolw[:, g, :],
                                                                  xb(MIXB + tc * 512, [[1, 512]]), start=True, stop=True),
                      reads=[("pl", "mix"), ("pl", "mix2"), "poolw"], writes=[("bank", bi)])
                P.add("act", lambda e, g=g, tc=tc, bi=bi: e.mul(
                    xb(ZTO + g * 2048 + tc * 512, [[1, 512]]), bk(bi, 0, 512), pscale[:, g:g + 1]),
                    reads=[("bank", bi), "pscale"], writes=[("zT", g, tc)])

    def merge_and_out():
        P.add("pool", lambda e: e.memset(fscr[:, 2:3], 0.0), reads=[("zT", g, tc) for g in range(4) for tc in range(NTC)],
              writes=["Xep"])
        for c in range(8):
            s0 = ring_use("win", 16 + c)
            s1 = ring_use("win", 24 + c)
            sb = ring_use("bpba", c)
            for tc in range(NTC):
                b0, b1, byp, bya = nbank("all"), nbank("all"), nbank("all"), nbank("all")
                ukeys = [("uT", 4 * tc + i) for i in range(4)]

                def mm0(e, s0=s0, tc=tc, b0=b0):
                    ins = None
                    for k in range(8):
                        ins = e.matmul(bk(b0, 0, 512), ring[:, s0, k, :], uT[:, k, tc * 512:(tc + 1) * 512],
                                       start=(k == 0), stop=(k == 7))
                    return ins

                def mm1(e, s1=s1, tc=tc, b1=b1):
                    ins = None
                    for k in range(8):
                        ins = e.matmul(bk(b1, 0, 512), ring[:, s1, k, :], uT[:, k, tc * 512:(tc + 1) * 512],
                                       start=(k == 0), stop=(k == 7))
                    return ins

                def mmyp(e, sb=sb, tc=tc, byp=byp):
                    ins = None
                    for g in range(4):
                        ins = e.matmul(bk(byp, 0, 512), ring[:, sb, g, :], xb(ZTO + g * 2048 + tc * 512, [[1, 512]]),
                                       start=(g == 0), stop=(g == 3))
                    return ins

                def mmya(e, sb=sb, tc=tc, bya=bya):
                    ins = None
                    for j in range(4):
                        ins = e.matmul(bk(bya, 0, 512), ring[:, sb, 4 + j, :], xb(OTO + j * 2048 + tc * 512, [[1, 512]]),
                                       start=(j == 0), stop=(j == 3))
                    return ins
                P.add("pe", mm0, reads=rk(s0) + ukeys, writes=[("bank", b0)])
                P.add("pe", mm1, reads=rk(s1) + ukeys, writes=[("bank", b1)])
                P.add("pe", mmyp, reads=rk(sb) + [("zT", g, tc) for g in range(4)], writes=[("bank", byp)])
                P.add("pe", mmya, reads=rk(sb) + [("OT", j, e_, tc) for j in range(4) for e_ in range(2)],
                      writes=[("bank", bya)])
                ta_, tb_ = 2 * (tc % 2), 2 * (tc % 2) + 1
                P.add("act", lambda e, b0=b0, ta_=ta_: e.activation(tmp[:, ta_, :], bk(b0, 0, 512), AF.Sigmoid),
                      reads=[("bank", b0)], writes=[("tmp", ta_)])
                P.add("act", lambda e, b1=b1, tb_=tb_: e.activation(tmp[:, tb_, :], bk(b1, 0, 512), AF.Sigmoid),
                      reads=[("bank", b1)], writes=[("tmp", tb_)])
                P.add("dve", lambda e, byp=byp, ta_=ta_: e.tensor_tensor(tmp[:, ta_, :], tmp[:, ta_, :], bk(byp, 0, 512), ALU.mult),
                      reads=[("tmp", ta_), ("bank", byp)], writes=[("tmp", ta_)])
                P.add("dve", lambda e, bya=bya, tb_=tb_: e.tensor_tensor(tmp[:, tb_, :], tmp[:, tb_, :], bk(bya, 0, 512), ALU.mult),
                      reads=[("tmp", tb_), ("bank", bya)], writes=[("tmp", tb_)])
                P.add("dve", lambda e, ta_=ta_, tb_=tb_, c=c, tc=tc: e.tensor_tensor(
                    xb(MRG + c * 2048 + tc * 512, [[1, 512]]), tmp[:, ta_, :], tmp[:, tb_, :], ALU.add),
                    reads=[("tmp", ta_), ("tmp", tb_)], writes=[("mrg", c, tc)])
        if debug:
            P.dma("pool", lambda e: e.dma_start(out=dbg_mrg, in_=xb(MRG, [[2048, 8], [1, 2048]])),
                  reads=[("mrg", c, tc) for c in range(8) for tc in range(NTC)], sem_key="dbg_mrg", is_out=True)
        P.add("pool", lambda e: e.memset(fscr[:, 3:4], 0.0), reads=[("mrg", c, tc) for c in range(8) for tc in range(NTC)],
              writes=[("OT", j, e_, qc) for j in range(4) for e_ in range(2) for qc in range(NTC)])
        P.dma("pool", lambda e: e.dma_start(out=xb(WOUT, [[1024, 8], [1, 1024]]),
                                            in_=wout_d.rearrange("(k p) n -> p k n", p=128)),
              reads=[("OT", j, e_, qc) for j in range(4) for e_ in range(2) for qc in range(NTC)],
              writes=[("wout", 0)], sem_key="wout")
        for t in range(NT):
            for hf in range(2):
                bi = nbank("all")

                def mmo(e, t=t, hf=hf, bi=bi):
                    ins = None
                    for c in range(8):
                        ins = e.matmul(bk(bi, 0, 512), xb(MRG + c * 2048 + t * 128, [[1, 128]]),
                                       xb(WOUT + c * 1024 + hf * 512, [[1, 512]]), start=(c == 0), stop=(c == 7))
                    return ins
                P.add("pe", mmo, reads=[("mrg", c, t // 4) for c in range(8)] + [("wout", 0)], writes=[("bank", bi)])
                P.add("dve", lambda e, t=t, hf=hf, bi=bi: e.tensor_tensor(
                    h[:, t, hf * 512:(hf + 1) * 512], bk(bi, 0, 512), h[:, t, hf * 512:(hf + 1) * 512], ALU.add),
                    reads=[("bank", bi), ("h", t, hf)], writes=[("h", t, hf)])

    def final_norm(s):
        fence()
        norm_stats(3)
        for t in range(NT):
            b = t % 2
            P.add("dve", lambda e, t=t, b=b: e.scalar_tensor_tensor(
                xf(OBUF_F + b * 1024, [[1, 1024]]), h[:, t, :], rstd[:, t:t + 1], gain[:], ALU.mult, ALU.mult),
                reads=[("h", t, 0), ("h", t, 1), "rstd", "gain"], writes=[("obuf", b)])
            P.dma("sp", lambda e, t=t, b=b: e.dma_start(out=out_d[s, t * 128:(t + 1) * 128, :],
                                                         in_=xf(OBUF_F + b * 1024, [[1, 1024]])),
                  reads=[("obuf", b)], sem_key=("out", b), is_out=True)

    def dump(i):
        if not debug:
            return
        P.dma("sp", lambda e, i=i: e.dma_start(out=dbg_d[i].rearrange("(t p) d -> p t d", p=128), in_=h[:]),
              reads=[("h", t, hf) for t in range(NT) for hf in range(2)], sem_key=("dbg", i), is_out=True)

    for s in range(nseq):
        load_x(s)
        ffn("w1g", "w1u", w1d_d, norm_to_uT(0))
        dump(0)
        attention(norm_to_uT(1))
        if debug:
            P.dma("pool", lambda e: e.dma_start(out=dbg_ot, in_=xb(OTO, [[2048, 4], [1, 2048]])),
                  reads=[("OT", j, e_, qc) for j in range(4) for e_ in range(2) for qc in range(NTC)], sem_key="dbg_ot", is_out=True)
        pool_mixer()
        if debug:
            P.dma("pool", lambda e: e.dma_start(out=dbg_zt, in_=xb(ZTO, [[2048, 4], [1, 2048]])),
                  reads=[("zT", g, tc) for g in range(4) for tc in range(NTC)], sem_key="dbg_zt", is_out=True)
        merge_and_out()
        dump(1)
        ffn("w2g", "w2u", w2d_d, norm_to_uT(2))
        dump(2)
        final_norm(s)

    P.resolve()
    with contextlib.ExitStack() as st:
        sems = {}
        for i, sn in enumerate(P.sem_names()):
            sems[sn] = st.enter_context(nc.semaphore(f"s{i}"))
        P.emit(nc, sems)
    return nc


def _tile_cols(w):
    K, N = w.shape
    return np.ascontiguousarray(w.reshape(K // 128, 128, N // 128, 128).transpose(2, 1, 0, 3))


def _consts():
    f = np.float32
    ident = np.eye(128, dtype=f)
    i = np.arange(128)
    tri = np.where(i[:, None] > i[None, :], f(MBIG), f(0.0)).astype(f)
    t = np.arange(S)
    slopes = np.exp2(-np.arange(1, 9, dtype=np.float64)).astype(f)
    alibiq = np.zeros((8, 2, S), f)
    for hh in range(8):
        alibiq[hh, 0] = -8.0 * slopes[hh] * ((t // 64) * 64)
        alibiq[hh, 1] = -8.0 * slopes[hh] * (t % 64)
    kaaug = np.zeros((10, S), f)
    for n in range(8):
        kaaug[n] = (t // 256 == n)
    kaaug[8:] = 1.0
    expb = np.zeros((128, 8, 16), f)
    for hh in range(8):
        for kt in range(16):
            expb[:, hh, kt] = slopes[hh] * (kt * 128 + i)
    invc = np.zeros((128, 4, 16), f)
    for g in range(4):
        w = 2 ** (g + 1)
        invc[:, g, :] = 1.0 / np.minimum(np.arange(16) + 1, w)
    return dict(ident=ident, tri=tri, alibiq=alibiq, alibik=-alibiq, kaaug=kaaug, expb=expb, invc=invc)


_NC_CACHE = {}


def kernel(x, ffn1_norm, ffn1_w_gate, ffn1_w_up, ffn1_w_down, mix_norm, w_in,
           pool_w, pool_scale, w_branch_pool, w_branch_attn, w_out,
           ffn2_norm, ffn2_w_gate, ffn2_w_up, ffn2_w_down, final_norm):
    f = np.float32
    x = np.asarray(x, f)
    A = lambda a: np.asarray(a, f)
    shared = dict(
        w1g=_tile_cols(A(ffn1_w_gate)[0]), w1u=_tile_cols(A(ffn1_w_up)[0]),
        w2g=_tile_cols(A(ffn2_w_gate)[0]), w2u=_tile_cols(A(ffn2_w_up)[0]),
        win=_tile_cols(A(w_in)[0]),
        wbp=_tile_cols(A(w_branch_pool)[0]), wba=_tile_cols(A(w_branch_attn)[0]),
        w1d=np.ascontiguousarray(A(ffn1_w_down)[0]), w2d=np.ascontiguousarray(A(ffn2_w_down)[0]),
        wout=np.ascontiguousarray(A(w_out)[0]),
        poolw=np.ascontiguousarray(A(pool_w)[0].transpose(1, 0, 2)),
        gains=np.ascontiguousarray(np.broadcast_to(
            np.stack([A(ffn1_norm)[0], A(mix_norm)[0], A(ffn2_norm)[0], A(final_norm)])[:, None, :], (4, 128, D))),
        pscale=np.ascontiguousarray(A(pool_scale)[0].reshape(4, 128).T),
    )
    shared.update(_consts())
    if "nc" not in _NC_CACHE:
        _NC_CACHE["nc"] = build_program(NSEQ)
    nc = _NC_CACHE["nc"]
    in_maps = []
    for c in range(NCORES):
        m = dict(shared)
        m["x"] = np.ascontiguousarray(x[c * NSEQ:(c + 1) * NSEQ])
        in_maps.append(m)
    res = run_bass_kernel_spmd(nc, in_maps, core_ids=list(range(NCORES)))
    return np.concatenate([np.asarray(r["out"], f) for r in res.results], axis=0)
```

```python
import contextlib
import numpy as np
import concourse.bass as bass
import concourse.mybir as mybir
from concourse.bass_utils import run_bass_kernel_spmd

F32 = mybir.dt.float32
BF16 = mybir.dt.bfloat16
AF = mybir.ActivationFunctionType
ALU = mybir.AluOpType
AX = mybir.AxisListType

NCORES = 8
NSEQ = 4
S = 2048
D = 1024
DFF = 2816
NCH = DFF // 128
NT = S // 128
NTC = S // 512
EPS = 1e-6
MBIG = -240000.0
FILL = True
ENGS = ("pe", "act", "dve", "pool", "sp")
XNAMES = {"hT", "wd", "QA", "KA", "V", "PT", "OT", "zT", "gat", "pl", "mrg", "wout", "obuf"}


class Op:
    __slots__ = ("eng", "fn", "reads", "writes", "is_dma", "sem_key", "waits",
                 "signal", "need_signal", "idx", "is_out")

    def __init__(self, eng, fn, reads, writes, is_dma=False, sem_key=None, is_out=False):
        self.eng = eng
        self.fn = fn
        self.reads = tuple(reads)
        self.writes = tuple(writes)
        self.is_dma = is_dma
        self.sem_key = sem_key
        self.waits = []
        self.signal = None
        self.need_signal = False
        self.is_out = is_out


def _isx(k):
    return isinstance(k, tuple) and k[0] in XNAMES


class Prog:
    def __init__(self):
        self.ops = []
        self.final_waits = {}

    def _fix(self, reads, writes):
        reads = list(reads)
        if any(_isx(k) for k in reads) or any(_isx(k) for k in writes):
            reads.append("Xep")
        return reads, list(writes)

    def add(self, eng, fn, reads=(), writes=()):
        reads, writes = self._fix(reads, writes)
        op = Op(eng, fn, reads, writes)
        self.ops.append(op)
        return op

    def dma(self, queue, fn, reads=(), writes=(), sem_key=None, is_out=False):
        reads, writes = self._fix(reads, writes)
        op = Op(queue, fn, reads, writes, is_dma=True, sem_key=sem_key, is_out=is_out)
        self.ops.append(op)
        return op

    def resolve(self):
        last_writer = {}
        readers = {}
        deps_of = []
        for i, op in enumerate(self.ops):
            op.idx = i
            deps = set()
            for k in op.reads:
                w = last_writer.get(k)
                if w is not None:
                    deps.add(w)
            for k in op.writes:
                w = last_writer.get(k)
                if w is not None:
                    deps.add(w)
                for r in readers.get(k, ()):
                    deps.add(r)
            for k in op.reads:
                readers.setdefault(k, []).append(i)
            for k in op.writes:
                last_writer[k] = i
                readers[k] = []
            deps.discard(i)
            real = []
            for d in deps:
                dop = self.ops[d]
                if dop.is_dma:
                    real.append(d)
                    continue
                if dop.eng == op.eng:
                    if op.is_dma:
                        real.append(d)
                        continue
                    if op.eng == "pe":
                        continue
                    real.append(d)
                    continue
                real.append(d)
            deps_of.append(real)
            for d in real:
                self.ops[d].need_signal = True
        for op in self.ops:
            if op.is_dma and op.is_out:
                op.need_signal = True
        counters = {}
        for op in self.ops:
            if op.is_dma:
                sn = ("dma", op.sem_key)
                counters[sn] = counters.get(sn, 0) + 16
                op.signal = (sn, counters[sn])
            elif op.need_signal:
                sn = ("eng", op.eng)
                counters[sn] = counters.get(sn, 0) + 1
                op.signal = (sn, counters[sn])
        self.counters = counters
        waited = {e: {} for e in ENGS}
        for i, op in enumerate(self.ops):
            need = {}
            for d in deps_of[i]:
                sn, v = self.ops[d].signal
                if v > need.get(sn, 0):
                    need[sn] = v
            for sn, v in need.items():
                if waited[op.eng].get(sn, 0) >= v:
                    continue
                waited[op.eng][sn] = v
                op.waits.append((sn, v))
        for op in self.ops:
            if op.is_dma and op.is_out:
                sn, v = op.signal
                cur = self.final_waits.setdefault(op.eng, {})
                if v > cur.get(sn, 0):
                    cur[sn] = v
        return counters

    def sem_names(self):
        return sorted(self.counters.keys(), key=str)

    def emit(self, nc, sems):
        by_eng = {e: [] for e in ENGS}
        for op in self.ops:
            by_eng[op.eng].append(op)

        def run(engobj, ename):
            for op in by_eng[ename]:
                for sn, v in op.waits:
                    engobj.wait_ge(sems[sn], v)
                ins = op.fn(engobj)
                if op.signal is not None:
                    sn, v = op.signal
                    if op.is_dma:
                        ins.then_inc(sems[sn], 16)
                    elif op.need_signal:
                        ins.then_inc(sems[sn], 1)
            for sn, v in self.final_waits.get(ename, {}).items():
                engobj.wait_ge(sems[sn], v)

        with nc.Block() as block:
            @block.tensor
            def _(e):
                run(e, "pe")

            @block.scalar
            def _(e):
                run(e, "act")

            @block.vector
            def _(e):
                run(e, "dve")

            @block.gpsimd
            def _(e):
                run(e, "pool")

            @block.sync
            def _(e):
                run(e, "sp")


def build_program(nseq=NSEQ, debug=False):
    nc = bass.Bass("TRN2", target_bir_lowering=False)
    dbg_d = nc.dram_tensor("dbg", [4, S, D], F32, kind="ExternalOutput").ap() if debug else None
    dbg_ot = nc.dram_tensor("dbg_ot", [128, 4, S], F32, kind="ExternalOutput").ap() if debug else None
    dbg_zt = nc.dram_tensor("dbg_zt", [128, 4, S], F32, kind="ExternalOutput").ap() if debug else None
    dbg_mrg = nc.dram_tensor("dbg_mrg", [128, 8, S], F32, kind="ExternalOutput").ap() if debug else None

    def din(name, shape):
        return nc.dram_tensor(name, list(shape), F32, kind="ExternalInput").ap()

    x_d = din("x", [nseq, S, D])
    out_d = nc.dram_tensor("out", [nseq, S, D], F32, kind="ExternalOutput").ap()
    wgt = {
        "w1g": din("w1g", [NCH, 128, 8, 128]), "w1u": din("w1u", [NCH, 128, 8, 128]),
        "w2g": din("w2g", [NCH, 128, 8, 128]), "w2u": din("w2u", [NCH, 128, 8, 128]),
        "win": din("win", [32, 128, 8, 128]),
        "wbp": din("wbp", [8, 128, 4, 128]), "wba": din("wba", [8, 128, 4, 128]),
    }
    w1d_d = din("w1d", [DFF, D])
    w2d_d = din("w2d", [DFF, D])
    wout_d = din("wout", [D, D])
    poolw_d = din("poolw", [128, 4, 128])
    gains_d = din("gains", [4, 128, D])
    pscale_d = din("pscale", [128, 4])
    ident_d = din("ident", [128, 128])
    tri_d = din("tri", [128, 128])
    alibi_d = din("alibiq", [8, 2, S])
    alibik_d = din("alibik", [8, 2, S])
    kaaug_d = din("kaaug", [10, S])
    expb_d = din("expb", [128, 8, 16])
    invc_d = din("invc", [128, 4, 16])

    h = nc.alloc_sbuf_tensor("h", [128, NT, D], F32)
    uT = nc.alloc_sbuf_tensor("uT", [128, 8, S], BF16)
    ring = nc.alloc_sbuf_tensor("ring", [128, 8, 8, 128], BF16)
    gain = nc.alloc_sbuf_tensor("gain", [128, D], F32)
    ub = nc.alloc_sbuf_tensor("ub", [128, 2, D], BF16)
    junk = nc.alloc_sbuf_tensor("junk", [128, D], BF16)
    tmp = nc.alloc_sbuf_tensor("tmp", [128, 4, 512], F32)
    ss = nc.alloc_sbuf_tensor("ss", [128, NT], F32)
    rstd = nc.alloc_sbuf_tensor("rstd", [128, NT], F32)
    ident = nc.alloc_sbuf_tensor("ident_s", [128, 128], BF16)
    tri = nc.alloc_sbuf_tensor("tri_s", [128, 128], BF16)
    expb = nc.alloc_sbuf_tensor("expb_s", [128, 8, 16], F32)
    invc = nc.alloc_sbuf_tensor("invc_s", [128, 4, 16], F32)
    pscale = nc.alloc_sbuf_tensor("pscale_s", [128, 4], F32)
    poolw = nc.alloc_sbuf_tensor("poolw_s", [128, 4, 128], BF16)
    fscr = nc.alloc_sbuf_tensor("fscr", [128, 8], F32)
    XR = 32768
    XB = nc.alloc_sbuf_tensor("X", [128, XR], BF16)
    XF = XB.bitcast(F32)
    XRF = XR // 2
    ps = nc.alloc_psum_tensor("ps", [128, 4096], F32)
    psb = ps.bitcast(BF16)
    rec = nc.alloc_sbuf_tensor("rec", [128, 512], F32)

    def xb(off, dims, p0=0, np_=128):
        return bass.AP(XB, p0 * XR + off, [[XR, np_]] + [list(d) for d in dims])

    def xf(off, dims, p0=0, np_=128):
        return bass.AP(XF, p0 * XRF + off, [[XRF, np_]] + [list(d) for d in dims])

    def bk(i, c0, n, p0=0, np_=128):
        return bass.AP(ps, p0 * 4096 + i * 512 + c0, [[4096, np_], [1, n]])

    HT = lambda b: b * 8192
    WD = lambda b: 16384 + b * 4096
    QA = lambda e: e * 2048
    KA = lambda e: 4096 + e * 2048
    VO = 8192
    PT = lambda i: 11264 + i * 1024
    GCP_F = 7168
    CMP_F = 7296
    RANK_F = 7424
    MBP = 14880
    KMS_F = 8016
    KMB = 16064
    OTO = 16384
    ZTO = 24576
    PB_F, TA_F, TB_F = 0, 2064, 4128
    MIXB = 12384
    MRG = 0
    WOUT = 16384
    OBUF_F = 0

    P = Prog()
    att_state = {"sp": 0}
    pools = {"main": [0, 1, 2, 3, 4, 5], "acc": [6, 7], "all": [0, 1, 2, 3, 4, 5, 6, 7], "aux": [4, 5]}
    pool_ctr = {"main": 0, "acc": 0, "all": 0, "aux": 0}

    def nbank(pool="main"):
        lst = pools[pool]
        b = lst[pool_ctr[pool] % len(lst)]
        pool_ctr[pool] += 1
        return b

    def fence():
        P.add("pool", lambda e: e.memset(fscr[:, 0:1], 0.0), reads=[], writes=["Xep"])

    uses = []
    for s in range(nseq):
        for c in range(NCH):
            uses += [("w1g", c), ("w1u", c)]
        for j in range(4):
            uses += [("win", 8 + j), ("win", 12 + j), ("win", 4 + j)]
        for g in range(4):
            uses += [("win", g)]
        for c in range(8):
            uses += [("win", 16 + c), ("win", 24 + c), ("bpba", c)]
        for c in range(NCH):
            uses += [("w2g", c), ("w2u", c)]
    ring_state = {"emitted": 0, "used": 0}
    LOOKAHEAD = 5

    def ring_emit_upto(n):
        while ring_state["emitted"] < min(n, len(uses)):
            m = ring_state["emitted"]
            kind, idx = uses[m]
            slot = m % 8
            if kind == "bpba":
                P.dma("pool", lambda e, slot=slot, idx=idx: e.dma_start(out=ring[:, slot, 0:4, :], in_=wgt["wbp"][idx]),
                      writes=[("ring", slot)], sem_key=("ring", slot, 0))
                P.dma("pool", lambda e, slot=slot, idx=idx: e.dma_start(out=ring[:, slot, 4:8, :], in_=wgt["wba"][idx]),
                      writes=[("ringb", slot)], reads=[("ring", slot)], sem_key=("ring", slot, 1))
            else:
                P.dma("pool", lambda e, slot=slot, kind=kind, idx=idx: e.dma_start(out=ring[:, slot, :, :], in_=wgt[kind][idx]),
                      writes=[("ring", slot), ("ringb", slot)], sem_key=("ring", slot, 0))
            ring_state["emitted"] += 1

    def ring_use(kind, idx):
        m = ring_state["used"]
        assert uses[m] == (kind, idx), (uses[m], kind, idx)
        ring_emit_upto(m + 1 + LOOKAHEAD)
        ring_state["used"] += 1
        return m % 8

    def rk(slot):
        return [("ring", slot), ("ringb", slot)]

    P.dma("pool", lambda e: e.dma_start(out=ident[:], in_=ident_d), writes=["ident"], sem_key="c_ident")
    P.dma("pool", lambda e: e.dma_start(out=tri[:], in_=tri_d), writes=["tri"], sem_key="c_tri")
    P.dma("pool", lambda e: e.dma_start(out=poolw[:], in_=poolw_d), writes=["poolw"], sem_key="c_poolw")
    P.dma("sp", lambda e: e.dma_start(out=expb[:], in_=expb_d), writes=["expb"], sem_key="c_expb")
    P.dma("sp", lambda e: e.dma_start(out=invc[:], in_=invc_d), writes=["invc"], sem_key="c_invc")
    P.dma("sp", lambda e: e.dma_start(out=pscale[:], in_=pscale_d), writes=["pscale"], sem_key="c_pscale")

    def load_x_tile(s, t):
        P.dma("sp", lambda e, t=t, s=s: e.dma_start(out=h[:, t, :], in_=x_d[s, t * 128:(t + 1) * 128, :]),
              writes=[("h", t, 0), ("h", t, 1)], sem_key=("x", t))

    def load_x(s):
        for t in range(NT):
            load_x_tile(s, t)

    def norm_stats(gidx):
        P.dma("sp", lambda e: e.dma_start(out=gain[:], in_=gains_d[gidx]), writes=["gain"], sem_key="gain")
        P.add("act", lambda e: e.memzero(ss[:]), writes=[("ss", t) for t in range(NT)])
        for t in range(NT):
            P.add("act", lambda e, t=t: e.activation(junk[:], h[:, t, :], AF.Square, accum_out=ss[:, t:t + 1]),
                  reads=[("h", t, 0), ("h", t, 1), ("ss", t)], writes=["junk", ("ss", t)])
        allss = [("ss", t) for t in range(NT)]
        P.add("dve", lambda e: e.tensor_scalar(rstd[:], ss[:], 1.0 / D, EPS, ALU.mult, ALU.add),
              reads=allss, writes=["rstd"])
        P.add("act", lambda e: e.activation(rstd[:], rstd[:], AF.Sqrt), reads=["rstd"], writes=["rstd"])
        P.add("dve", lambda e: e.reciprocal(rstd[:], rstd[:]), reads=["rstd"], writes=["rstd"])

    def norm_to_uT(gidx):
        norm_stats(gidx)

        def emit_tiles(t0):
            for t in range(t0, t0 + 4):
                b = t % 2
                P.add("dve", lambda e, t=t, b=b: e.scalar_tensor_tensor(ub[:, b, :], h[:, t, :], rstd[:, t:t + 1], gain[:],
                                                                        ALU.mult, ALU.mult),
                      reads=[("h", t, 0), ("h", t, 1), "rstd", "gain"], writes=[("ub", b)])
                bi = nbank("all")

                def tr(e, b=b, bi=bi):
                    ins = None
                    for k in range(8):
                        ins = e.transpose(bass.AP(psb, bi * 1024 + k * 128, [[8192, 128], [1, 128]]),
                                          ub[:, b, k * 128:(k + 1) * 128], ident[:])
                    return ins
                P.add("pe", tr, reads=[("ub", b), "ident"], writes=[("bank", bi)])
                P.add("act", lambda e, t=t, bi=bi: e.activation(
                    bass.AP(uT, t * 128, [[8 * S, 128], [S, 8], [1, 128]]),
                    bass.AP(psb, bi * 1024, [[8192, 128], [128, 8], [1, 128]]), AF.Copy),
                    reads=[("bank", bi)], writes=[("uT", t)])
        return [lambda t0=t0: emit_tiles(t0) for t0 in (0, 4, 8, 12)]

    def ffn(gk, uk, wd_d, pending):
        parts = [(0, 4), (4, 8), (8, 12), (12, 16), (16, 20), (20, 22)]
        fence()

        def gu(pi):
            c0, c1 = parts[pi]
            hb = pi % 2
            P.dma("pool", lambda e, c0=c0, c1=c1, hb=hb: e.dma_start(
                out=xb(WD(hb), [[1024, c1 - c0], [1, 1024]]),
                in_=wd_d[c0 * 128:c1 * 128, :].rearrange("(c p) n -> p c n", p=128)),
                writes=[("wd", hb)], sem_key=("wd", hb))
            for c in range(c0, c1):
                sg = ring_use(gk, c)
                su = ring_use(uk, c)
                for tc in range(NTC):
                    if pending and tc == 0:
                        pending.pop(0)()
                    if pending:
                        pending.pop(0)()
                    bg = nbank("all")
                    bu = nbank("all")

                    def mm(e, sg=sg, su=su, tc=tc, bg=bg, bu=bu):
                        ins = None
                        for k in range(8):
                            ins = e.matmul(bk(bg, 0, 512), ring[:, sg, k, :], uT[:, k, tc * 512:(tc + 1) * 512],
                                           start=(k == 0), stop=(k == 7))
                        for k in range(8):
                            ins = e.matmul(bk(bu, 0, 512), ring[:, su, k, :], uT[:, k, tc * 512:(tc + 1) * 512],
                                           start=(k == 0), stop=(k == 7))
                        return ins
                    P.add("pe", mm, reads=rk(sg) + rk(su) + [("uT", 4 * tc + i) for i in range(4)],
                          writes=[("bank", bg), ("bank", bu)])
                    tb_ = (c * NTC + tc) % 4
                    P.add("act", lambda e, bg=bg, tb_=tb_: e.activation(tmp[:, tb_, :], bk(bg, 0, 512), AF.Silu),
                          reads=[("bank", bg)], writes=[("tmp", tb_)])
                    P.add("dve", lambda e, bu=bu, tb_=tb_, hb=hb, cc=c - c0, tc=tc: e.tensor_tensor(
                        xb(HT(hb) + cc * 2048 + tc * 512, [[1, 512]]), tmp[:, tb_, :], bk(bu, 0, 512), ALU.mult),
                        reads=[("tmp", tb_), ("bank", bu)], writes=[("hT", hb, c - c0, tc)])

        def down(pi):
            c0, c1 = parts[pi]
            hb = pi % 2
            n = c1 - c0
            for t in range(NT):
                for hf in range(2):
                    bi = nbank("all")

                    def mm(e, t=t, hf=hf, bi=bi, n=n, hb=hb):
                        ins = None
                        for cc in range(n):
                            ins = e.matmul(bk(bi, 0, 512), xb(HT(hb) + cc * 2048 + t * 128, [[1, 128]]),
                                           xb(WD(hb) + cc * 1024 + hf * 512, [[1, 512]]),
                                           start=(cc == 0), stop=(cc == n - 1))
                        return ins
                    P.add("pe", mm, reads=[("hT", hb, cc, t // 4) for cc in range(n)] + [("wd", hb)],
                          writes=[("bank", bi)])
                    P.add("dve", lambda e, t=t, hf=hf, bi=bi: e.scalar_tensor_tensor(
                        h[:, t, hf * 512:(hf + 1) * 512], bk(bi, 0, 512), 0.5, h[:, t, hf * 512:(hf + 1) * 512],
                        ALU.mult, ALU.add),
                        reads=[("bank", bi), ("h", t, hf)], writes=[("h", t, hf)])

        gu(0)
        for pi in range(1, len(parts)):
            gu(pi)
            down(pi - 1)
        down(len(parts) - 1)

    def attention(pending):
        fence()
        for e_ in range(2):
            P.dma("pool", lambda e, e_=e_: e.dma_start(out=xb(KA(e_), [[1, S]], p0=64, np_=10), in_=kaaug_d),
                  writes=[("KA", e_, "aug")], sem_key=("kaaug", e_))
            P.dma("pool", lambda e, e_=e_: e.dma_start(out=xb(QA(e_), [[1, S]], p0=74, np_=2), in_=kaaug_d[8:10, :]),
                  writes=[("QA", e_, "ones")], sem_key=("qaones", e_))
        P.add("pool", lambda e: e.memset(xb(MBP, [[1, 8 * 2 * 72]]), 0.0), writes=[("gat", "mbp")])
        P.add("pool", lambda e: e.memset(xb(VO, [[1, 16 * 192]]), 1.0), writes=[("V", "all")])
        for j in range(4):
            sk = ring_use("win", 8 + j)
            sv = ring_use("win", 12 + j)
            sq = ring_use("win", 4 + j)
            for e_ in range(2):
                hh = 2 * j + e_
                P.add("pool", lambda e, e_=e_: e.memset(xb(QA(e_), [[1, S]], p0=64, np_=8), 0.0),
                      writes=[("QA", e_, "mask")])
                P.dma("pool", lambda e, e_=e_, hh=hh: e.dma_start(out=xb(QA(e_), [[1, S]], p0=72, np_=2), in_=alibi_d[hh]),
                      writes=[("QA", e_, "alibi")], sem_key=("alibi", e_))
                P.dma("pool", lambda e, e_=e_, hh=hh: e.dma_start(out=xb(KA(e_), [[1, S]], p0=74, np_=2), in_=alibik_d[hh]),
                      writes=[("KA", e_, "kpos")], sem_key=("alibik", e_))
            for tc in range(NTC):
                if pending and tc == 0:
                    pending.pop(0)()
                if pending:
                    pending.pop(0)()
                bi = nbank("aux")

                def mmk(e, sk=sk, tc=tc, bi=bi):
                    ins = None
                    for k in range(8):
                        ins = e.matmul(bk(bi, 0, 512), ring[:, sk, k, :], uT[:, k, tc * 512:(tc + 1) * 512],
                                       start=(k == 0), stop=(k == 7))
                    return ins
                P.add("pe", mmk, reads=rk(sk) + [("uT", 4 * tc + i) for i in range(4)], writes=[("bank", bi)])
                for e_ in range(2):
                    P.add("dve", lambda e, tc=tc, bi=bi, e_=e_: e.tensor_copy(
                        xb(KA(e_) + tc * 512, [[1, 512]], np_=64), bk(bi, 0, 512, p0=64 * e_, np_=64)),
                        reads=[("bank", bi)], writes=[("KA", e_, tc)])
                    P.add("dve", lambda e, tc=tc, bi=bi, e_=e_: e.tensor_reduce(
                        xf(KMS_F + e_ * 8 + 2 * tc, [[1, 2]], np_=64),
                        bass.AP(ps, e_ * 64 * 4096 + bi * 512, [[4096, 64], [256, 2], [1, 256]]), AX.X, ALU.add),
                        reads=[("bank", bi)], writes=[("gat", "kms", e_, tc)])
            P.add("dve", lambda e: e.tensor_scalar(xb(KMB, [[1, 16]], np_=64), xf(KMS_F, [[1, 16]], np_=64),
                                                   1.0 / 256, 0.0, ALU.mult, ALU.add),
                  reads=[("gat", "kms", e_, tc) for e_ in range(2) for tc in range(NTC)], writes=[("gat", "kmb")])
            for t4 in range(4):
                bi = nbank("aux")

                def mmv(e, sv=sv, t4=t4, bi=bi):
                    ins = None
                    for tt in range(4):
                        t = t4 * 4 + tt
                        for k in range(8):
                            ins = e.matmul(bk(bi, tt * 128, 128), uT[:, k, t * 128:(t + 1) * 128], ring[:, sv, k, :],
                                           start=(k == 0), stop=(k == 7))
                    return ins
                P.add("pe", mmv, reads=rk(sv) + [("uT", t4 * 4 + i) for i in range(4)], writes=[("bank", bi)])
                P.add("dve", lambda e, t4=t4, bi=bi: e.tensor_copy(
                    xb(VO + t4 * 4 * 192, [[192, 4], [128, 2], [1, 64]]),
                    bass.AP(ps, bi * 512, [[4096, 128], [128, 4], [64, 2], [1, 64]])),
                    reads=[("bank", bi), ("V", "all")], writes=[("V", t4)])
            for tc in range(NTC):
                bi = nbank("aux")

                def mmq(e, sq=sq, tc=tc, bi=bi):
                    ins = None
                    for k in range(8):
                        ins = e.matmul(bk(bi, 0, 512), ring[:, sq, k, :], uT[:, k, tc * 512:(tc + 1) * 512],
                                       start=(k == 0), stop=(k == 7))
                    return ins
                P.add("pe", mmq, reads=rk(sq) + [("uT", 4 * tc + i) for i in range(4)], writes=[("bank", bi)])
                for e_ in range(2):
                    P.add("dve", lambda e, tc=tc, bi=bi, e_=e_: e.tensor_copy(
                        xb(QA(e_) + tc * 512, [[1, 512]], np_=64), bk(bi, 0, 512, p0=64 * e_, np_=64)),
                        reads=[("bank", bi)], writes=[("QA", e_, tc)])
            bg = nbank("aux")

            def mmg(e, bg=bg):
                ins = None
                for t8 in range(8):
                    t = 8 + t8
                    for e_ in range(2):
                        ins = e.matmul(bk(bg, (t8 * 2 + e_) * 8, 8), xb(QA(e_) + t * 128, [[1, 128]], np_=64),
                                       xb(KMB + e_ * 8, [[1, 8]], np_=64), start=True, stop=True)
                return ins
            P.add("pe", mmg, reads=[("QA", e_, tc) for e_ in range(2) for tc in (2, 3)] + [("gat", "kmb")],
                  writes=[("bank", bg)])
            P.add("dve", lambda e, bg=bg: e.tensor_copy(xf(GCP_F, [[1, 128]]), bk(bg, 0, 128)),
                  reads=[("bank", bg)], writes=[("gat", "gcp")])
            for t8 in range(8):
                n = (8 + t8) // 2
                gj = xf(GCP_F + t8 * 16, [[8, 2], [0, n], [1, n]])
                gi = xf(GCP_F + t8 * 16, [[8, 2], [1, n], [0, n]])
                P.add("dve", lambda e, gj=gj, gi=gi, n=n: e.tensor_tensor(xf(CMP_F, [[n * n, 2], [n, n], [1, n]]), gj, gi, ALU.is_gt),
                      reads=[("gat", "gcp")], writes=[("gat", "cmp")])
                P.add("dve", lambda e, n=n: e.tensor_reduce(xf(RANK_F, [[8, 2], [1, n]]),
                                                            xf(CMP_F, [[n * n, 2], [n, n], [1, n]]), AX.X, ALU.add),
                      reads=[("gat", "cmp")], writes=[("gat", "rank")])
                P.add("dve", lambda e, n=n, t8=t8: e.tensor_scalar(
                    xb(MBP + t8 * 144 + 64, [[72, 2], [1, n]]), xf(RANK_F, [[8, 2], [1, n]]), 2.5, MBIG, ALU.is_ge, ALU.mult),
                    reads=[("gat", "rank"), ("gat", "mbp")], writes=[("gat", "mb", t8)])
            for e_ in range(2):
                for hf in range(2):
                    bi = nbank("aux")

                    def mmt(e, e_=e_, hf=hf, bi=bi):
                        ins = None
                        for tt in range(4):
                            t8 = hf * 4 + tt
                            ins = e.matmul(bk(bi, tt * 128, 128, np_=72), xb(MBP + t8 * 144 + e_ * 72, [[1, 72]]),
                                           ident[:], start=True, stop=True)
                        return ins
                    P.add("pe", mmt, reads=[("gat", "mb", hf * 4 + tt) for tt in range(4)] + ["ident"],
                          writes=[("bank", bi)])
                    P.add("dve", lambda e, e_=e_, hf=hf, bi=bi: e.tensor_copy(
                        xb(QA(e_) + 1024 + hf * 512, [[1, 512]], p0=64, np_=8), bk(bi, 0, 512, p0=64, np_=8)),
                        reads=[("bank", bi), ("QA", e_, "mask")], writes=[("QA", e_, "mask2", hf)])
            steps = []
            for e_ in range(2):
                for qc in range(NTC):
                    bo = nbank("acc")
                    nkt = 4 * qc + 4
                    for p in range(nkt // 2):
                        steps.append(dict(e_=e_, qc=qc, p=p, bo=bo, nkt=nkt, jj=j, last=(p == nkt // 2 - 1)))

            def s_step(st):
                e_, qc, p = st["e_"], st["qc"], st["p"]
                qa_keys = [("QA", e_, "mask"), ("QA", e_, "alibi"), ("QA", e_, "ones"),
                           ("QA", e_, "mask2", 0), ("QA", e_, "mask2", 1)]
                ka_keys = [("KA", e_, "aug"), ("KA", e_, "kpos")]
                sp = att_state["sp"] % 2
                pp = att_state["sp"] % 3
                att_state["sp"] += 1
                kt0 = 2 * p
                r0 = kt0 - 4 * qc
                c0 = 128 * r0 if r0 > 0 else 0

                def mms(e, c0=c0, sp=sp, kt0=kt0, e_=e_, qc=qc):
                    ins = None
                    for i in range(2):
                        kt = kt0 + i
                        r = kt - 4 * qc
                        b = 2 * sp + i
                        ins = e.matmul(bk(b, c0, 512 - c0), xb(KA(e_) + kt * 128, [[1, 128]], np_=76),
                                       xb(QA(e_) + qc * 512 + c0, [[1, 512 - c0]], np_=76),
                                       start=True, stop=(r < 0))
                        if r >= 0:
                            ins = e.matmul(bk(b, 128 * r, 128), ident[:], tri[:], start=False, stop=True)
                    return ins
                P.add("pe", mms, reads=[("KA", e_, kt0 // 4), ("QA", e_, qc), "ident", "tri"] + qa_keys + ka_keys,
                      writes=[("bank", 2 * sp), ("bank", 2 * sp + 1)])
                P.add("act", lambda e, c0=c0, sp=sp, pp=pp: e.activation(
                    xb(PT(pp) + c0, [[512, 2], [1, 512 - c0]]),
                    bass.AP(ps, 2 * sp * 512 + c0, [[4096, 128], [512, 2], [1, 512 - c0]]), AF.Exp, scale=0.125),
                    reads=[("bank", 2 * sp), ("bank", 2 * sp + 1)], writes=[("PT", pp)])
                return (st, kt0, pp)

            def pv_step(st, kt0, pp):
                e_, qc, bo, nkt, jj = st["e_"], st["qc"], st["bo"], st["nkt"], st["jj"]

                def mmpv(e):
                    ins = None
                    for i in range(2):
                        kt = kt0 + i
                        r = kt - 4 * qc
                        c0i = 128 * r if r > 0 else 0
                        ins = e.matmul(bk(bo, c0i, 512 - c0i), xb(VO + kt * 192 + e_ * 64, [[1, 128]]),
                                       xb(PT(pp) + i * 512 + c0i, [[1, 512 - c0i]]),
                                       start=(kt == 0), stop=(kt == nkt - 1))
                    return ins
                P.add("pe", mmpv, reads=[("PT", pp), ("V", kt0 // 4), ("V", "all")], writes=[("bank", bo)])
                if FILL:
                    P.add("pe", lambda e: e.matmul(bk(5, 0, 384), ident[:], uT[:, 0, 0:384], start=True, stop=True),
                          reads=[], writes=[("bank", 5)])
                if st["last"]:
                    if e_ == 0:
                        P.add("dve", lambda e: e.reciprocal(rec[0:64, :], bk(bo, 0, 512, p0=64, np_=64)),
                              reads=[("bank", bo)], writes=["rec"])
                        P.add("dve", lambda e: e.tensor_tensor(
                            xb(OTO + jj * 2048 + qc * 512, [[1, 512]], np_=64), bk(bo, 0, 512, np_=64),
                            rec[0:64, :], ALU.mult),
                            reads=[("bank", bo), "rec"], writes=[("OT", jj, 0, qc)])
                    else:
                        P.add("dve", lambda e: e.reciprocal(rec[64:128, :], bk(bo, 0, 512, np_=64)),
                              reads=[("bank", bo)], writes=["rec"])
                        P.add("dve", lambda e: e.tensor_tensor(
                            xb(OTO + jj * 2048 + qc * 512, [[1, 512]], p0=64, np_=64), bk(bo, 0, 512, p0=64, np_=64),
                            rec[64:128, :], ALU.mult),
                            reads=[("bank", bo), "rec"], writes=[("OT", jj, 1, qc)])

            prev = None
            for st in steps:
                cur = s_step(st)
                if prev is not None:
                    pv_step(*prev)
                prev = cur
            pv_step(*prev)

    def pool_mixer():
        P.add("pool", lambda e: e.memset(fscr[:, 1:2], 0.0), reads=[("OT", j, e_, qc) for j in range(4) for e_ in range(2) for qc in range(NTC)],
              writes=["Xep"])
        for off in (PB_F, TA_F, TB_F):
            P.add("pool", lambda e, off=off: e.memset(xf(off, [[1, 16]]), 0.0), writes=[("pl", "pad", off)])
        for g in range(4):
            sp_ = ring_use("win", g)
            w = 2 ** (g + 1)
            for tc in range(NTC):
                bi = nbank()

                def mmp(e, sp_=sp_, tc=tc, bi=bi):
                    ins = None
                    for k in range(8):
                        ins = e.matmul(bk(bi, 0, 512), ring[:, sp_, k, :], uT[:, k, tc * 512:(tc + 1) * 512],
                                       start=(k == 0), stop=(k == 7))
                    return ins
                P.add("pe", mmp, reads=rk(sp_) + [("uT", 4 * tc + i) for i in range(4)], writes=[("bank", bi)])
                P.add("act", lambda e, tc=tc, bi=bi: e.activation(xf(PB_F + 16 + tc * 512, [[1, 512]]), bk(bi, 0, 512), AF.Copy),
                      reads=[("bank", bi), ("pl", "pad", PB_F)], writes=[("pl", "p", tc)])
            pkeys = [("pl", "p", tc) for tc in range(NTC)]
            src = PB_F
            dsts = [TA_F, TB_F]
            for i in range(g + 1):
                sh = 2 ** i
                dst = dsts[i % 2]
                P.add("pool", lambda e, src=src, dst=dst, sh=sh: e.tensor_tensor(
                    xf(dst + 16, [[1, S]]), xf(src + 16, [[1, S]]), xf(src + 16 - sh, [[1, S]]), ALU.add),
                    reads=pkeys + [("pl", "sum", src), ("pl", "pad", src)], writes=[("pl", "sum", dst), ("pl", "pad", dst)])
                src = dst
            P.add("dve", lambda e, src=src, w=w: e.scalar_tensor_tensor(
                xb(MIXB, [[1, S]]), xf(src + 16, [[1, S]]), 1.0 / w, xf(PB_F + 16, [[1, S]]), ALU.mult, ALU.subtract),
                reads=pkeys + [("pl", "sum", src)], writes=[("pl", "mix")])
            P.add("pool", lambda e, src=src, g=g: e.tensor_tensor(
                xf(TA_F if src == TB_F else TB_F, [[1, 16]]), xf(src + 16, [[1, 16]]), invc[:, g, :], ALU.mult),
                reads=[("pl", "sum", src), "invc"], writes=[("pl", "pad", TA_F if src == TB_F else TB_F), ("pl", "fix")])
            P.add("pool", lambda e, src=src: e.tensor_tensor(
                xb(MIXB, [[1, 16]]), xf(TA_F if src == TB_F else TB_F, [[1, 16]]), xf(PB_F + 16, [[1, 16]]), ALU.subtract),
                reads=[("pl", "fix"), ("pl", "mix")] + pkeys, writes=[("pl", "mix2")])
            P.add("pool", lambda e, src=src: e.memset(xf(TA_F if src == TB_F else TB_F, [[1, 16]]), 0.0),
                  reads=[("pl", "mix2")], writes=[("pl", "pad", TA_F if src == TB_F else TB_F), ("pl", "fix")])
            for tc in range(NTC):
                bi = nbank()
                P.add("pe", lambda e, g=g, tc=tc, bi=bi: e.matmul(bk(bi, 0, 512), poolw[:, g, :],
                                                                  xb(MIXB + tc * 512, [[1, 512]]), start=True, stop=True),
                      reads=[("pl", "mix"), ("pl", "mix2"), "poolw"], writes=[("bank", bi)])
                P.add("act", lambda e, g=g, tc=tc, bi=bi: e.mul(
                    xb(ZTO + g * 2048 + tc * 512, [[1, 512]]), bk(bi, 0, 512), pscale[:, g:g + 1]),
                    reads=[("bank", bi), "pscale"], writes=[("zT", g, tc)])

    def merge_and_out():
        P.add("pool", lambda e: e.memset(fscr[:, 2:3], 0.0), reads=[("zT", g, tc) for g in range(4) for tc in range(NTC)],
              writes=["Xep"])
        for c in range(8):
            s0 = ring_use("win", 16 + c)
            s1 = ring_use("win", 24 + c)
            sb = ring_use("bpba", c)
            for tc in range(NTC):
                b0, b1, byp, bya = nbank("all"), nbank("all"), nbank("all"), nbank("all")
                ukeys = [("uT", 4 * tc + i) for i in range(4)]

                def mm0(e, s0=s0, tc=tc, b0=b0):
                    ins = None
                    for k in range(8):
                        ins = e.matmul(bk(b0, 0, 512), ring[:, s0, k, :], uT[:, k, tc * 512:(tc + 1) * 512],
                                       start=(k == 0), stop=(k == 7))
                    return ins

                def mm1(e, s1=s1, tc=tc, b1=b1):
                    ins = None
                    for k in range(8):
                        ins = e.matmul(bk(b1, 0, 512), ring[:, s1, k, :], uT[:, k, tc * 512:(tc + 1) * 512],
                                       start=(k == 0), stop=(k == 7))
                    return ins

                def mmyp(e, sb=sb, tc=tc, byp=byp):
                    ins = None
                    for g in range(4):
                        ins = e.matmul(bk(byp, 0, 512), ring[:, sb, g, :], xb(ZTO + g * 2048 + tc * 512, [[1, 512]]),
                                       start=(g == 0), stop=(g == 3))
                    return ins

                def mmya(e, sb=sb, tc=tc, bya=bya):
                    ins = None
                    for j in range(4):
                        ins = e.matmul(bk(bya, 0, 512), ring[:, sb, 4 + j, :], xb(OTO + j * 2048 + tc * 512, [[1, 512]]),
                                       start=(j == 0), stop=(j == 3))
                    return ins
                P.add("pe", mm0, reads=rk(s0) + ukeys, writes=[("bank", b0)])
                P.add("pe", mm1, reads=rk(s1) + ukeys, writes=[("bank", b1)])
                P.add("pe", mmyp, reads=rk(sb) + [("zT", g, tc) for g in range(4)], writes=[("bank", byp)])
                P.add("pe", mmya, reads=rk(sb) + [("OT", j, e_, tc) for j in range(4) for e_ in range(2)],
                      writes=[("bank", bya)])
                ta_, tb_ = 2 * (tc % 2), 2 * (tc % 2) + 1
                P.add("act", lambda e, b0=b0, ta_=ta_: e.activation(tmp[:, ta_, :], bk(b0, 0, 512), AF.Sigmoid),
                      reads=[("bank", b0)], writes=[("tmp", ta_)])
                P.add("act", lambda e, b1=b1, tb_=tb_: e.activation(tmp[:, tb_, :], bk(b1, 0, 512), AF.Sigmoid),
                      reads=[("bank", b1)], writes=[("tmp", tb_)])
                P.add("dve", lambda e, byp=byp, ta_=ta_: e.tensor_tensor(tmp[:, ta_, :], tmp[:, ta_, :], bk(byp, 0, 512), ALU.mult),
                      reads=[("tmp", ta_), ("bank", byp)], writes=[("tmp", ta_)])
                P.add("dve", lambda e, bya=bya, tb_=tb_: e.tensor_tensor(tmp[:, tb_, :], tmp[:, tb_, :], bk(bya, 0, 512), ALU.mult),
                      reads=[("tmp", tb_), ("bank", bya)], writes=[("tmp", tb_)])
                P.add("dve", lambda e, ta_=ta_, tb_=tb_, c=c, tc=tc: e.tensor_tensor(
                    xb(MRG + c * 2048 + tc * 512, [[1, 512]]), tmp[:, ta_, :], tmp[:, tb_, :], ALU.add),
                    reads=[("tmp", ta_), ("tmp", tb_)], writes=[("mrg", c, tc)])
        if debug:
            P.dma("pool", lambda e: e.dma_start(out=dbg_mrg, in_=xb(MRG, [[2048, 8], [1, 2048]])),
                  reads=[("mrg", c, tc) for c in range(8) for tc in range(NTC)], sem_key="dbg_mrg", is_out=True)
        P.add("pool", lambda e: e.memset(fscr[:, 3:4], 0.0), reads=[("mrg", c, tc) for c in range(8) for tc in range(NTC)],
              writes=[("OT", j, e_, qc) for j in range(4) for e_ in range(2) for qc in range(NTC)])
        P.dma("pool", lambda e: e.dma_start(out=xb(WOUT, [[1024, 8], [1, 1024]]),
                                            in_=wout_d.rearrange("(k p) n -> p k n", p=128)),
              reads=[("OT", j, e_, qc) for j in range(4) for e_ in range(2) for qc in range(NTC)],
              writes=[("wout", 0)], sem_key="wout")
        for t in range(NT):
            for hf in range(2):
                bi = nbank("all")

                def mmo(e, t=t, hf=hf, bi=bi):
                    ins = None
                    for c in range(8):
                        ins = e.matmul(bk(bi, 0, 512), xb(MRG + c * 2048 + t * 128, [[1, 128]]),
                                       xb(WOUT + c * 1024 + hf * 512, [[1, 512]]), start=(c == 0), stop=(c == 7))
                    return ins
                P.add("pe", mmo, reads=[("mrg", c, t // 4) for c in range(8)] + [("wout", 0)], writes=[("bank", bi)])
                P.add("dve", lambda e, t=t, hf=hf, bi=bi: e.tensor_tensor(
                    h[:, t, hf * 512:(hf + 1) * 512], bk(bi, 0, 512), h[:, t, hf * 512:(hf + 1) * 512], ALU.add),
                    reads=[("bank", bi), ("h", t, hf)], writes=[("h", t, hf)])

    def final_norm(s, next_s=None):
        fence()
        norm_stats(3)
        for t in range(NT):
            b = t % 2
            P.add("dve", lambda e, t=t, b=b: e.scalar_tensor_tensor(
                xf(OBUF_F + b * 1024, [[1, 1024]]), h[:, t, :], rstd[:, t:t + 1], gain[:], ALU.mult, ALU.mult),
                reads=[("h", t, 0), ("h", t, 1), "rstd", "gain"], writes=[("obuf", b)])
            P.dma("sp", lambda e, t=t, b=b: e.dma_start(out=out_d[s, t * 128:(t + 1) * 128, :],
                                                         in_=xf(OBUF_F + b * 1024, [[1, 1024]])),
                  reads=[("obuf", b)], sem_key=("out", b), is_out=True)
            if next_s is not None:
                load_x_tile(next_s, t)

    def dump(i):
        if not debug:
            return
        P.dma("sp", lambda e, i=i: e.dma_start(out=dbg_d[i].rearrange("(t p) d -> p t d", p=128), in_=h[:]),
              reads=[("h", t, hf) for t in range(NT) for hf in range(2)], sem_key=("dbg", i), is_out=True)

    for s in range(nseq):
        if s == 0:
            load_x(s)
        ffn("w1g", "w1u", w1d_d, norm_to_uT(0))
        dump(0)
        attention(norm_to_uT(1))
        if debug:
            P.dma("pool", lambda e: e.dma_start(out=dbg_ot, in_=xb(OTO, [[2048, 4], [1, 2048]])),
                  reads=[("OT", j, e_, qc) for j in range(4) for e_ in range(2) for qc in range(NTC)], sem_key="dbg_ot", is_out=True)
        pool_mixer()
        if debug:
            P.dma("pool", lambda e: e.dma_start(out=dbg_zt, in_=xb(ZTO, [[2048, 4], [1, 2048]])),
                  reads=[("zT", g, tc) for g in range(4) for tc in range(NTC)], sem_key="dbg_zt", is_out=True)
        merge_and_out()
        dump(1)
        ffn("w2g", "w2u", w2d_d, norm_to_uT(2))
        dump(2)
        final_norm(s, s + 1 if s + 1 < nseq else None)

    P.resolve()
    with contextlib.ExitStack() as st:
        sems = {}
        for i, sn in enumerate(P.sem_names()):
            sems[sn] = st.enter_context(nc.semaphore(f"s{i}"))
        P.emit(nc, sems)
    return nc


def _tile_cols(w):
    K, N = w.shape
    return np.ascontiguousarray(w.reshape(K // 128, 128, N // 128, 128).transpose(2, 1, 0, 3))


def _consts():
    f = np.float32
    ident = np.eye(128, dtype=f)
    i = np.arange(128)
    tri = np.where(i[:, None] > i[None, :], f(MBIG), f(0.0)).astype(f)
    t = np.arange(S)
    slopes = np.exp2(-np.arange(1, 9, dtype=np.float64)).astype(f)
    alibiq = np.zeros((8, 2, S), f)
    for hh in range(8):
        alibiq[hh, 0] = -8.0 * slopes[hh] * ((t // 64) * 64)
        alibiq[hh, 1] = -8.0 * slopes[hh] * (t % 64)
    kaaug = np.zeros((10, S), f)
    for n in range(8):
        kaaug[n] = (t // 256 == n)
    kaaug[8:] = 1.0
    expb = np.zeros((128, 8, 16), f)
    for hh in range(8):
        for kt in range(16):
            expb[:, hh, kt] = slopes[hh] * (kt * 128 + i)
    invc = np.zeros((128, 4, 16), f)
    for g in range(4):
        w = 2 ** (g + 1)
        invc[:, g, :] = 1.0 / np.minimum(np.arange(16) + 1, w)
    return dict(ident=ident, tri=tri, alibiq=alibiq, alibik=-alibiq, kaaug=kaaug, expb=expb, invc=invc)


_NC_CACHE = {}


def kernel(x, ffn1_norm, ffn1_w_gate, ffn1_w_up, ffn1_w_down, mix_norm, w_in,
           pool_w, pool_scale, w_branch_pool, w_branch_attn, w_out,
           ffn2_norm, ffn2_w_gate, ffn2_w_up, ffn2_w_down, final_norm):
    f = np.float32
    x = np.asarray(x, f)
    A = lambda a: np.asarray(a, f)
    shared = dict(
        w1g=_tile_cols(A(ffn1_w_gate)[0]), w1u=_tile_cols(A(ffn1_w_up)[0]),
        w2g=_tile_cols(A(ffn2_w_gate)[0]), w2u=_tile_cols(A(ffn2_w_up)[0]),
        win=_tile_cols(A(w_in)[0]),
        wbp=_tile_cols(A(w_branch_pool)[0]), wba=_tile_cols(A(w_branch_attn)[0]),
        w1d=np.ascontiguousarray(A(ffn1_w_down)[0]), w2d=np.ascontiguousarray(A(ffn2_w_down)[0]),
        wout=np.ascontiguousarray(A(w_out)[0]),
        poolw=np.ascontiguousarray(A(pool_w)[0].transpose(1, 0, 2)),
        gains=np.ascontiguousarray(np.broadcast_to(
            np.stack([A(ffn1_norm)[0], A(mix_norm)[0], A(ffn2_norm)[0], A(final_norm)])[:, None, :], (4, 128, D))),
        pscale=np.ascontiguousarray(A(pool_scale)[0].reshape(4, 128).T),
    )
    shared.update(_consts())
    if "nc" not in _NC_CACHE:
        _NC_CACHE["nc"] = build_program(NSEQ)
    nc = _NC_CACHE["nc"]
    in_maps = []
    for c in range(NCORES):
        m = dict(shared)
        m["x"] = np.ascontiguousarray(x[c * NSEQ:(c + 1) * NSEQ])
        in_maps.append(m)
    res = run_bass_kernel_spmd(nc, in_maps, core_ids=list(range(NCORES)))
    return np.concatenate([np.asarray(r["out"], f) for r in res.results], axis=0)
```

```python
import contextlib
import numpy as np
import concourse.bass as bass
import concourse.mybir as mybir
from concourse.bass_utils import run_bass_kernel_spmd

F32 = mybir.dt.float32
BF16 = mybir.dt.bfloat16
AF = mybir.ActivationFunctionType
ALU = mybir.AluOpType
AX = mybir.AxisListType

NCORES = 8
NSEQ = 4
S = 2048
D = 1024
DFF = 2816
NCH = DFF // 128
NT = S // 128
NTC = S // 512
EPS = 1e-6
MBIG = -240000.0
FILL = True
ENGS = ("pe", "act", "dve", "pool", "sp")
XNAMES = {"hT", "wd", "QA", "KA", "V", "PT", "OT", "zT", "gat", "pl", "mrg", "wout", "obuf"}


class Op:
    __slots__ = ("eng", "fn", "reads", "writes", "is_dma", "sem_key", "waits",
                 "signal", "need_signal", "idx", "is_out")

    def __init__(self, eng, fn, reads, writes, is_dma=False, sem_key=None, is_out=False):
        self.eng = eng
        self.fn = fn
        self.reads = tuple(reads)
        self.writes = tuple(writes)
        self.is_dma = is_dma
        self.sem_key = sem_key
        self.waits = []
        self.signal = None
        self.need_signal = False
        self.is_out = is_out


def _isx(k):
    return isinstance(k, tuple) and k[0] in XNAMES


class Prog:
    def __init__(self):
        self.ops = []
        self.final_waits = {}

    def _fix(self, reads, writes):
        reads = list(reads)
        if any(_isx(k) for k in reads) or any(_isx(k) for k in writes):
            reads.append("Xep")
        return reads, list(writes)

    def add(self, eng, fn, reads=(), writes=()):
        reads, writes = self._fix(reads, writes)
        op = Op(eng, fn, reads, writes)
        self.ops.append(op)
        return op

    def dma(self, queue, fn, reads=(), writes=(), sem_key=None, is_out=False):
        reads, writes = self._fix(reads, writes)
        op = Op(queue, fn, reads, writes, is_dma=True, sem_key=sem_key, is_out=is_out)
        self.ops.append(op)
        return op

    def resolve(self):
        last_writer = {}
        readers = {}
        deps_of = []
        for i, op in enumerate(self.ops):
            op.idx = i
            deps = set()
            for k in op.reads:
                w = last_writer.get(k)
                if w is not None:
                    deps.add(w)
            for k in op.writes:
                w = last_writer.get(k)
                if w is not None:
                    deps.add(w)
                for r in readers.get(k, ()):
                    deps.add(r)
            for k in op.reads:
                readers.setdefault(k, []).append(i)
            for k in op.writes:
                last_writer[k] = i
                readers[k] = []
            deps.discard(i)
            real = []
            for d in deps:
                dop = self.ops[d]
                if dop.is_dma:
                    real.append(d)
                    continue
                if dop.eng == op.eng:
                    if op.is_dma:
                        real.append(d)
                        continue
                    if op.eng == "pe":
                        continue
                    real.append(d)
                    continue
                real.append(d)
            deps_of.append(real)
            for d in real:
                self.ops[d].need_signal = True
        for op in self.ops:
            if op.is_dma and op.is_out:
                op.need_signal = True
        counters = {}
        for op in self.ops:
            if op.is_dma:
                sn = ("dma", op.sem_key)
                counters[sn] = counters.get(sn, 0) + 16
                op.signal = (sn, counters[sn])
            elif op.need_signal:
                sn = ("eng", op.eng)
                counters[sn] = counters.get(sn, 0) + 1
                op.signal = (sn, counters[sn])
        self.counters = counters
        waited = {e: {} for e in ENGS}
        for i, op in enumerate(self.ops):
            need = {}
            for d in deps_of[i]:
                sn, v = self.ops[d].signal
                if v > need.get(sn, 0):
                    need[sn] = v
            for sn, v in need.items():
                if waited[op.eng].get(sn, 0) >= v:
                    continue
                waited[op.eng][sn] = v
                op.waits.append((sn, v))
        for op in self.ops:
            if op.is_dma and op.is_out:
                sn, v = op.signal
                cur = self.final_waits.setdefault(op.eng, {})
                if v > cur.get(sn, 0):
                    cur[sn] = v
        return counters

    def sem_names(self):
        return sorted(self.counters.keys(), key=str)

    def emit(self, nc, sems):
        by_eng = {e: [] for e in ENGS}
        for op in self.ops:
            by_eng[op.eng].append(op)

        def run(engobj, ename):
            for op in by_eng[ename]:
                for sn, v in op.waits:
                    engobj.wait_ge(sems[sn], v)
                ins = op.fn(engobj)
                if op.signal is not None:
                    sn, v = op.signal
                    if op.is_dma:
                        ins.then_inc(sems[sn], 16)
                    elif op.need_signal:
                        ins.then_inc(sems[sn], 1)
            for sn, v in self.final_waits.get(ename, {}).items():
                engobj.wait_ge(sems[sn], v)

        with nc.Block() as block:
            @block.tensor
            def _(e):
                run(e, "pe")

            @block.scalar
            def _(e):
                run(e, "act")

            @block.vector
            def _(e):
                run(e, "dve")

            @block.gpsimd
            def _(e):
                run(e, "pool")

            @block.sync
            def _(e):
                run(e, "sp")


def build_program(nseq=NSEQ, debug=False):
    nc = bass.Bass("TRN2", target_bir_lowering=False)
    dbg_d = nc.dram_tensor("dbg", [4, S, D], F32, kind="ExternalOutput").ap() if debug else None
    dbg_ot = nc.dram_tensor("dbg_ot", [128, 4, S], F32, kind="ExternalOutput").ap() if debug else None
    dbg_zt = nc.dram_tensor("dbg_zt", [128, 4, S], F32, kind="ExternalOutput").ap() if debug else None
    dbg_mrg = nc.dram_tensor("dbg_mrg", [128, 8, S], F32, kind="ExternalOutput").ap() if debug else None

    def din(name, shape):
        return nc.dram_tensor(name, list(shape), F32, kind="ExternalInput").ap()

    x_d = din("x", [nseq, S, D])
    out_d = nc.dram_tensor("out", [nseq, S, D], F32, kind="ExternalOutput").ap()
    wgt = {
        "w1g": din("w1g", [NCH, 128, 8, 128]), "w1u": din("w1u", [NCH, 128, 8, 128]),
        "w2g": din("w2g", [NCH, 128, 8, 128]), "w2u": din("w2u", [NCH, 128, 8, 128]),
        "win": din("win", [32, 128, 8, 128]),
        "wbp": din("wbp", [8, 128, 4, 128]), "wba": din("wba", [8, 128, 4, 128]),
    }
    w1d_d = din("w1d", [DFF, D])
    w2d_d = din("w2d", [DFF, D])
    wout_d = din("wout", [D, D])
    poolw_d = din("poolw", [128, 4, 128])
    gains_d = din("gains", [4, 128, D])
    pscale_d = din("pscale", [128, 4])
    ident_d = din("ident", [128, 128])
    tri_d = din("tri", [128, 128])
    tri2_d = din("tri2", [128, 256])
    alibi_d = din("alibiq", [8, 2, S])
    alibik_d = din("alibik", [8, 2, S])
    kaaug_d = din("kaaug", [10, S])
    expb_d = din("expb", [128, 8, 16])
    invc_d = din("invc", [128, 4, 16])

    h = nc.alloc_sbuf_tensor("h", [128, NT, D], F32)
    uT = nc.alloc_sbuf_tensor("uT", [128, 8, S], BF16)
    ring = nc.alloc_sbuf_tensor("ring", [128, 8, 8, 128], BF16)
    gain = nc.alloc_sbuf_tensor("gain", [128, D], F32)
    ub = nc.alloc_sbuf_tensor("ub", [128, 2, D], BF16)
    junk = nc.alloc_sbuf_tensor("junk", [128, D], BF16)
    tmp = nc.alloc_sbuf_tensor("tmp", [128, 4, 512], F32)
    ss = nc.alloc_sbuf_tensor("ss", [128, NT], F32)
    rstd = nc.alloc_sbuf_tensor("rstd", [128, NT], F32)
    ident = nc.alloc_sbuf_tensor("ident_s", [128, 128], BF16)
    tri = nc.alloc_sbuf_tensor("tri_s", [128, 128], BF16)
    tri2 = nc.alloc_sbuf_tensor("tri2_s", [128, 256], BF16)
    expb = nc.alloc_sbuf_tensor("expb_s", [128, 8, 16], F32)
    invc = nc.alloc_sbuf_tensor("invc_s", [128, 4, 16], F32)
    pscale = nc.alloc_sbuf_tensor("pscale_s", [128, 4], F32)
    poolw = nc.alloc_sbuf_tensor("poolw_s", [128, 4, 128], BF16)
    fscr = nc.alloc_sbuf_tensor("fscr", [128, 8], F32)
    XR = 32768
    XB = nc.alloc_sbuf_tensor("X", [128, XR], BF16)
    XF = XB.bitcast(F32)
    XRF = XR // 2
    ps = nc.alloc_psum_tensor("ps", [128, 4096], F32)
    psb = ps.bitcast(BF16)
    rec = nc.alloc_sbuf_tensor("rec", [128, 512], F32)

    def xb(off, dims, p0=0, np_=128):
        return bass.AP(XB, p0 * XR + off, [[XR, np_]] + [list(d) for d in dims])

    def xf(off, dims, p0=0, np_=128):
        return bass.AP(XF, p0 * XRF + off, [[XRF, np_]] + [list(d) for d in dims])

    def bk(i, c0, n, p0=0, np_=128):
        return bass.AP(ps, p0 * 4096 + i * 512 + c0, [[4096, np_], [1, n]])

    HT = lambda b: b * 8192
    WD = lambda b: 16384 + b * 4096
    QA = lambda e: e * 2048
    KA = lambda e: 4096 + e * 2048
    VO = 8192
    PT = lambda i: 11264 + i * 1024
    GCP_F = 7168
    CMP_F = 7296
    RANK_F = 7424
    MBP = 14880
    KMS_F = 8016
    KMB = 16064
    OTO = 16384
    ZTO = 24576
    PB_F, TA_F, TB_F = 0, 2064, 4128
    MIXB = 12384
    MRG = 0
    WOUT = 16384
    OBUF_F = 0

    P = Prog()
    att_state = {"sp": 0}
    pools = {"main": [0, 1, 2, 3, 4, 5], "acc": [6, 7], "all": [0, 1, 2, 3, 4, 5, 6, 7], "aux": [4, 5]}
    pool_ctr = {"main": 0, "acc": 0, "all": 0, "aux": 0}

    def nbank(pool="main"):
        lst = pools[pool]
        b = lst[pool_ctr[pool] % len(lst)]
        pool_ctr[pool] += 1
        return b

    def fence():
        P.add("pool", lambda e: e.memset(fscr[:, 0:1], 0.0), reads=[], writes=["Xep"])

    uses = []
    for s in range(nseq):
        for c in range(NCH):
            uses += [("w1g", c), ("w1u", c)]
        for j in range(4):
            uses += [("win", 8 + j), ("win", 12 + j), ("win", 4 + j)]
        for g in range(4):
            uses += [("win", g)]
        for c in range(8):
            uses += [("win", 16 + c), ("win", 24 + c), ("bpba", c)]
        for c in range(8):
            uses += [("wout", c)]
        for c in range(NCH):
            uses += [("w2g", c), ("w2u", c)]
    ring_state = {"emitted": 0, "used": 0}
    LOOKAHEAD = 5

    def ring_emit_upto(n):
        while ring_state["emitted"] < min(n, len(uses)):
            m = ring_state["emitted"]
            kind, idx = uses[m]
            slot = m % 8
            if kind == "bpba":
                P.dma("pool", lambda e, slot=slot, idx=idx: e.dma_start(out=ring[:, slot, 0:4, :], in_=wgt["wbp"][idx]),
                      writes=[("ring", slot)], sem_key=("ring", slot, 0))
                P.dma("pool", lambda e, slot=slot, idx=idx: e.dma_start(out=ring[:, slot, 4:8, :], in_=wgt["wba"][idx]),
                      writes=[("ringb", slot)], reads=[("ring", slot)], sem_key=("ring", slot, 1))
            elif kind == "wout":
                P.dma("pool", lambda e, slot=slot, idx=idx: e.dma_start(
                    out=bass.AP(ring, slot * 1024, [[8192, 128], [1, 1024]]), in_=wout_d[idx * 128:(idx + 1) * 128, :]),
                    writes=[("ring", slot), ("ringb", slot)], sem_key=("ring", slot, 0))
            else:
                P.dma("pool", lambda e, slot=slot, kind=kind, idx=idx: e.dma_start(out=ring[:, slot, :, :], in_=wgt[kind][idx]),
                      writes=[("ring", slot), ("ringb", slot)], sem_key=("ring", slot, 0))
            ring_state["emitted"] += 1

    def ring_use(kind, idx, la=LOOKAHEAD):
        m = ring_state["used"]
        assert uses[m] == (kind, idx), (uses[m], kind, idx)
        ring_emit_upto(m + 1 + la)
        ring_state["used"] += 1
        return m % 8

    def rk(slot):
        return [("ring", slot), ("ringb", slot)]

    P.dma("pool", lambda e: e.dma_start(out=ident[:], in_=ident_d), writes=["ident"], sem_key="c_ident")
    P.dma("pool", lambda e: e.dma_start(out=tri[:], in_=tri_d), writes=["tri"], sem_key="c_tri")
    P.dma("pool", lambda e: e.dma_start(out=tri2[:], in_=tri2_d), writes=["tri2"], sem_key="c_tri2")
    P.dma("pool", lambda e: e.dma_start(out=poolw[:], in_=poolw_d), writes=["poolw"], sem_key="c_poolw")
    P.dma("sp", lambda e: e.dma_start(out=expb[:], in_=expb_d), writes=["expb"], sem_key="c_expb")
    P.dma("sp", lambda e: e.dma_start(out=invc[:], in_=invc_d), writes=["invc"], sem_key="c_invc")
    P.dma("sp", lambda e: e.dma_start(out=pscale[:], in_=pscale_d), writes=["pscale"], sem_key="c_pscale")

    def load_x_tile(s, t):
        P.dma("sp", lambda e, t=t, s=s: e.dma_start(out=h[:, t, :], in_=x_d[s, t * 128:(t + 1) * 128, :]),
              writes=[("h", t, 0), ("h", t, 1)], sem_key=("x", t))

    def load_x(s):
        for t in range(NT):
            load_x_tile(s, t)

    def norm_stats(gidx):
        P.dma("sp", lambda e: e.dma_start(out=gain[:], in_=gains_d[gidx]), writes=["gain"], sem_key="gain")
        P.add("act", lambda e: e.memzero(ss[:]), writes=[("ss", t) for t in range(NT)])
        for t in range(NT):
            P.add("act", lambda e, t=t: e.activation(junk[:], h[:, t, :], AF.Square, accum_out=ss[:, t:t + 1]),
                  reads=[("h", t, 0), ("h", t, 1), ("ss", t)], writes=["junk", ("ss", t)])
        allss = [("ss", t) for t in range(NT)]
        P.add("dve", lambda e: e.tensor_scalar(rstd[:], ss[:], 1.0 / D, EPS, ALU.mult, ALU.add),
              reads=allss, writes=["rstd"])
        P.add("act", lambda e: e.activation(rstd[:], rstd[:], AF.Sqrt), reads=["rstd"], writes=["rstd"])
        P.add("dve", lambda e: e.reciprocal(rstd[:], rstd[:]), reads=["rstd"], writes=["rstd"])

    def norm_to_uT(gidx):
        norm_stats(gidx)

        def emit_tiles(t0):
            for t in range(t0, t0 + 4):
                b = t % 2
                P.add("dve", lambda e, t=t, b=b: e.scalar_tensor_tensor(ub[:, b, :], h[:, t, :], rstd[:, t:t + 1], gain[:],
                                                                        ALU.mult, ALU.mult),
                      reads=[("h", t, 0), ("h", t, 1), "rstd", "gain"], writes=[("ub", b)])
                bi = nbank("all")

                def tr(e, b=b, bi=bi):
                    ins = None
                    for k in range(8):
                        ins = e.transpose(bass.AP(psb, bi * 1024 + k * 128, [[8192, 128], [1, 128]]),
                                          ub[:, b, k * 128:(k + 1) * 128], ident[:])
                    return ins
                P.add("pe", tr, reads=[("ub", b), "ident"], writes=[("bank", bi)])
                P.add("act", lambda e, t=t, bi=bi: e.activation(
                    bass.AP(uT, t * 128, [[8 * S, 128], [S, 8], [1, 128]]),
                    bass.AP(psb, bi * 1024, [[8192, 128], [128, 8], [1, 128]]), AF.Copy),
                    reads=[("bank", bi)], writes=[("uT", t)])
        return [lambda t0=t0: emit_tiles(t0) for t0 in (0, 4, 8, 12)]

    def ffn(gk, uk, wd_d, pending):
        parts = [(0, 4), (4, 8), (8, 12), (12, 16), (16, 20), (20, 22)]
        fence()

        def gu(pi):
            c0, c1 = parts[pi]
            hb = pi % 2
            P.dma("pool", lambda e, c0=c0, c1=c1, hb=hb: e.dma_start(
                out=xb(WD(hb), [[1024, c1 - c0], [1, 1024]]),
                in_=wd_d[c0 * 128:c1 * 128, :].rearrange("(c p) n -> p c n", p=128)),
                writes=[("wd", hb)], sem_key=("wd", hb))
            for c in range(c0, c1):
                sg = ring_use(gk, c)
                su = ring_use(uk, c)
                for tc in range(NTC):
                    if pending and tc == 0:
                        pending.pop(0)()
                    if pending:
                        pending.pop(0)()
                    bg = nbank("all")
                    bu = nbank("all")

                    def mm(e, sg=sg, su=su, tc=tc, bg=bg, bu=bu):
                        ins = None
                        for k in range(8):
                            ins = e.matmul(bk(bg, 0, 512), ring[:, sg, k, :], uT[:, k, tc * 512:(tc + 1) * 512],
                                           start=(k == 0), stop=(k == 7))
                        for k in range(8):
                            ins = e.matmul(bk(bu, 0, 512), ring[:, su, k, :], uT[:, k, tc * 512:(tc + 1) * 512],
                                           start=(k == 0), stop=(k == 7))
                        return ins
                    P.add("pe", mm, reads=rk(sg) + rk(su) + [("uT", 4 * tc + i) for i in range(4)],
                          writes=[("bank", bg), ("bank", bu)])
                    tb_ = (c * NTC + tc) % 4
                    P.add("act", lambda e, bg=bg, tb_=tb_: e.activation(tmp[:, tb_, :], bk(bg, 0, 512), AF.Silu),
                          reads=[("bank", bg)], writes=[("tmp", tb_)])
                    P.add("dve", lambda e, bu=bu, tb_=tb_, hb=hb, cc=c - c0, tc=tc: e.tensor_tensor(
                        xb(HT(hb) + cc * 2048 + tc * 512, [[1, 512]]), tmp[:, tb_, :], bk(bu, 0, 512), ALU.mult),
                        reads=[("tmp", tb_), ("bank", bu)], writes=[("hT", hb, c - c0, tc)])

        def down(pi):
            c0, c1 = parts[pi]
            hb = pi % 2
            n = c1 - c0
            for t in range(NT):
                for hf in range(2):
                    bi = nbank("all")

                    def mm(e, t=t, hf=hf, bi=bi, n=n, hb=hb):
                        ins = None
                        for cc in range(n):
                            ins = e.matmul(bk(bi, 0, 512), xb(HT(hb) + cc * 2048 + t * 128, [[1, 128]]),
                                           xb(WD(hb) + cc * 1024 + hf * 512, [[1, 512]]),
                                           start=(cc == 0), stop=(cc == n - 1))
                        return ins
                    P.add("pe", mm, reads=[("hT", hb, cc, t // 4) for cc in range(n)] + [("wd", hb)],
                          writes=[("bank", bi)])
                    P.add("dve", lambda e, t=t, hf=hf, bi=bi: e.scalar_tensor_tensor(
                        h[:, t, hf * 512:(hf + 1) * 512], bk(bi, 0, 512), 0.5, h[:, t, hf * 512:(hf + 1) * 512],
                        ALU.mult, ALU.add),
                        reads=[("bank", bi), ("h", t, hf)], writes=[("h", t, hf)])

        gu(0)
        for pi in range(1, len(parts)):
            gu(pi)
            down(pi - 1)
        down(len(parts) - 1)

    def attention(pending):
        fence()
        for e_ in range(2):
            P.dma("pool", lambda e, e_=e_: e.dma_start(out=xb(KA(e_), [[1, S]], p0=64, np_=10), in_=kaaug_d),
                  writes=[("KA", e_, "aug")], sem_key=("kaaug", e_))
            P.dma("pool", lambda e, e_=e_: e.dma_start(out=xb(QA(e_), [[1, S]], p0=74, np_=2), in_=kaaug_d[8:10, :]),
                  writes=[("QA", e_, "ones")], sem_key=("qaones", e_))
        P.add("pool", lambda e: e.memset(xb(MBP, [[1, 8 * 2 * 72]]), 0.0), writes=[("gat", "mbp")])
        P.add("pool", lambda e: e.memset(xb(VO, [[1, 16 * 192]]), 1.0), writes=[("V", "all")])
        for j in range(4):
            sk = ring_use("win", 8 + j)
            sv = ring_use("win", 12 + j)
            sq = ring_use("win", 4 + j)
            for e_ in range(2):
                hh = 2 * j + e_
                P.add("pool", lambda e, e_=e_: e.memset(xb(QA(e_), [[1, S]], p0=64, np_=8), 0.0),
                      writes=[("QA", e_, "mask")])
                P.dma("pool", lambda e, e_=e_, hh=hh: e.dma_start(out=xb(QA(e_), [[1, S]], p0=72, np_=2), in_=alibi_d[hh]),
                      writes=[("QA", e_, "alibi")], sem_key=("alibi", e_))
                P.dma("pool", lambda e, e_=e_, hh=hh: e.dma_start(out=xb(KA(e_), [[1, S]], p0=74, np_=2), in_=alibik_d[hh]),
                      writes=[("KA", e_, "kpos")], sem_key=("alibik", e_))
            for tc in range(NTC):
                if pending and tc == 0:
                    pending.pop(0)()
                if pending:
                    pending.pop(0)()
                bi = nbank("aux")

                def mmk(e, sk=sk, tc=tc, bi=bi):
                    ins = None
                    for k in range(8):
                        ins = e.matmul(bk(bi, 0, 512), ring[:, sk, k, :], uT[:, k, tc * 512:(tc + 1) * 512],
                                       start=(k == 0), stop=(k == 7))
                    return ins
                P.add("pe", mmk, reads=rk(sk) + [("uT", 4 * tc + i) for i in range(4)], writes=[("bank", bi)])
                for e_ in range(2):
                    P.add("dve", lambda e, tc=tc, bi=bi, e_=e_: e.tensor_copy(
                        xb(KA(e_) + tc * 512, [[1, 512]], np_=64), bk(bi, 0, 512, p0=64 * e_, np_=64)),
                        reads=[("bank", bi)], writes=[("KA", e_, tc)])
                    P.add("dve", lambda e, tc=tc, bi=bi, e_=e_: e.tensor_reduce(
                        xf(KMS_F + e_ * 8 + 2 * tc, [[1, 2]], np_=64),
                        bass.AP(ps, e_ * 64 * 4096 + bi * 512, [[4096, 64], [256, 2], [1, 256]]), AX.X, ALU.add),
                        reads=[("bank", bi)], writes=[("gat", "kms", e_, tc)])
            P.add("dve", lambda e: e.tensor_scalar(xb(KMB, [[1, 16]], np_=64), xf(KMS_F, [[1, 16]], np_=64),
                                                   1.0 / 256, 0.0, ALU.mult, ALU.add),
                  reads=[("gat", "kms", e_, tc) for e_ in range(2) for tc in range(NTC)], writes=[("gat", "kmb")])
            for t4 in range(4):
                bi = nbank("aux")

                def mmv(e, sv=sv, t4=t4, bi=bi):
                    ins = None
                    for tt in range(4):
                        t = t4 * 4 + tt
                        for k in range(8):
                            ins = e.matmul(bk(bi, tt * 128, 128), uT[:, k, t * 128:(t + 1) * 128], ring[:, sv, k, :],
                                           start=(k == 0), stop=(k == 7))
                    return ins
                P.add("pe", mmv, reads=rk(sv) + [("uT", t4 * 4 + i) for i in range(4)], writes=[("bank", bi)])
                P.add("dve", lambda e, t4=t4, bi=bi: e.tensor_copy(
                    xb(VO + t4 * 4 * 192, [[192, 4], [128, 2], [1, 64]]),
                    bass.AP(ps, bi * 512, [[4096, 128], [128, 4], [64, 2], [1, 64]])),
                    reads=[("bank", bi), ("V", "all")], writes=[("V", t4)])
            for tc in range(NTC):
                bi = nbank("aux")

                def mmq(e, sq=sq, tc=tc, bi=bi):
                    ins = None
                    for k in range(8):
                        ins = e.matmul(bk(bi, 0, 512), ring[:, sq, k, :], uT[:, k, tc * 512:(tc + 1) * 512],
                                       start=(k == 0), stop=(k == 7))
                    return ins
                P.add("pe", mmq, reads=rk(sq) + [("uT", 4 * tc + i) for i in range(4)], writes=[("bank", bi)])
                for e_ in range(2):
                    P.add("dve", lambda e, tc=tc, bi=bi, e_=e_: e.tensor_copy(
                        xb(QA(e_) + tc * 512, [[1, 512]], np_=64), bk(bi, 0, 512, p0=64 * e_, np_=64)),
                        reads=[("bank", bi)], writes=[("QA", e_, tc)])
            bg = nbank("aux")

            def mmg(e, bg=bg):
                ins = None
                for t8 in range(8):
                    t = 8 + t8
                    for e_ in range(2):
                        ins = e.matmul(bk(bg, (t8 * 2 + e_) * 8, 8), xb(QA(e_) + t * 128, [[1, 128]], np_=64),
                                       xb(KMB + e_ * 8, [[1, 8]], np_=64), start=True, stop=True)
                return ins
            P.add("pe", mmg, reads=[("QA", e_, tc) for e_ in range(2) for tc in (2, 3)] + [("gat", "kmb")],
                  writes=[("bank", bg)])
            P.add("dve", lambda e, bg=bg: e.tensor_copy(xf(GCP_F, [[1, 128]]), bk(bg, 0, 128)),
                  reads=[("bank", bg)], writes=[("gat", "gcp")])
            for t8 in range(8):
                n = (8 + t8) // 2
                gj = xf(GCP_F + t8 * 16, [[8, 2], [0, n], [1, n]])
                gi = xf(GCP_F + t8 * 16, [[8, 2], [1, n], [0, n]])
                P.add("dve", lambda e, gj=gj, gi=gi, n=n: e.tensor_tensor(xf(CMP_F, [[n * n, 2], [n, n], [1, n]]), gj, gi, ALU.is_gt),
                      reads=[("gat", "gcp")], writes=[("gat", "cmp")])
                P.add("dve", lambda e, n=n: e.tensor_reduce(xf(RANK_F, [[8, 2], [1, n]]),
                                                            xf(CMP_F, [[n * n, 2], [n, n], [1, n]]), AX.X, ALU.add),
                      reads=[("gat", "cmp")], writes=[("gat", "rank")])
                P.add("dve", lambda e, n=n, t8=t8: e.tensor_scalar(
                    xb(MBP + t8 * 144 + 64, [[72, 2], [1, n]]), xf(RANK_F, [[8, 2], [1, n]]), 2.5, MBIG, ALU.is_ge, ALU.mult),
                    reads=[("gat", "rank"), ("gat", "mbp")], writes=[("gat", "mb", t8)])
            def emit_mask_rows():
                for e_ in range(2):
                    for hf in range(2):
                        bi = nbank("aux")

                        def mmt(e, e_=e_, hf=hf, bi=bi):
                            ins = None
                            for tt in range(4):
                                t8 = hf * 4 + tt
                                ins = e.matmul(bk(bi, tt * 128, 128, np_=72), xb(MBP + t8 * 144 + e_ * 72, [[1, 72]]),
                                               ident[:], start=True, stop=True)
                            return ins
                        P.add("pe", mmt, reads=[("gat", "mb", hf * 4 + tt) for tt in range(4)] + ["ident"],
                              writes=[("bank", bi)])
                        P.add("dve", lambda e, e_=e_, hf=hf, bi=bi: e.tensor_copy(
                            xb(QA(e_) + 1024 + hf * 512, [[1, 512]], p0=64, np_=8), bk(bi, 0, 512, p0=64, np_=8)),
                            reads=[("bank", bi), ("QA", e_, "mask")], writes=[("QA", e_, "mask2", hf)])

            steps = []
            for gi_, (e_, qc) in enumerate([(0, 0), (0, 1), (1, 0), (1, 1), (0, 2), (0, 3), (1, 2), (1, 3)]):
                bo = nbank("acc")
                nkt = 4 * qc + 4
                for p in range(nkt // 2):
                    steps.append(dict(e_=e_, qc=qc, p=p, bo=bo, nkt=nkt, jj=j, last=(p == nkt // 2 - 1),
                                      need_mask=(gi_ == 4 and p == 0)))

            def s_step(st):
                e_, qc, p = st["e_"], st["qc"], st["p"]
                qa_keys = [("QA", e_, "mask"), ("QA", e_, "alibi"), ("QA", e_, "ones")]
                if qc >= 2:
                    qa_keys += [("QA", e_, "mask2", qc - 2)]
                ka_keys = [("KA", e_, "aug"), ("KA", e_, "kpos")]
                sp = att_state["sp"] % 2
                pp = att_state["sp"] % 3
                att_state["sp"] += 1
                kt0 = 2 * p
                r0 = kt0 - 4 * qc
                c0 = 128 * r0 if r0 > 0 else 0

                def mms(e, c0=c0, sp=sp, kt0=kt0, e_=e_, qc=qc):
                    ins = None
                    for i in range(2):
                        kt = kt0 + i
                        r = kt - 4 * qc
                        b = 2 * sp + i
                        ins = e.matmul(bk(b, c0, 512 - c0), xb(KA(e_) + kt * 128, [[1, 128]], np_=76),
                                       xb(QA(e_) + qc * 512 + c0, [[1, 512 - c0]], np_=76),
                                       start=True, stop=(r < 0))
                        if r >= 0 and i == 0:
                            ins = e.matmul(bk(b, 128 * r, 128), ident[:], tri[:], start=False, stop=True)
                        elif r >= 0:
                            ins = e.matmul(bk(b, c0, 256), ident[:], tri2[:], start=False, stop=True)
                    return ins
                P.add("pe", mms, reads=[("KA", e_, kt0 // 4), ("QA", e_, qc), "ident", "tri", "tri2"] + qa_keys + ka_keys,
                      writes=[("bank", 2 * sp), ("bank", 2 * sp + 1)])
                P.add("act", lambda e, c0=c0, sp=sp, pp=pp: e.activation(
                    xb(PT(pp) + c0, [[512, 2], [1, 512 - c0]]),
                    bass.AP(ps, 2 * sp * 512 + c0, [[4096, 128], [512, 2], [1, 512 - c0]]), AF.Exp, scale=0.125),
                    reads=[("bank", 2 * sp), ("bank", 2 * sp + 1)], writes=[("PT", pp)])
                return (st, kt0, pp)

            def pv_step(st, kt0, pp):
                e_, qc, bo, nkt, jj = st["e_"], st["qc"], st["bo"], st["nkt"], st["jj"]

                def mmpv(e):
                    ins = None
                    for i in range(2):
                        kt = kt0 + i
                        r = kt - 4 * qc
                        c0i = 128 * r if r > 0 else 0
                        ins = e.matmul(bk(bo, c0i, 512 - c0i), xb(VO + kt * 192 + e_ * 64, [[1, 128]]),
                                       xb(PT(pp) + i * 512 + c0i, [[1, 512 - c0i]]),
                                       start=(kt == 0), stop=(kt == nkt - 1))
                    return ins
                P.add("pe", mmpv, reads=[("PT", pp), ("V", kt0 // 4), ("V", "all")], writes=[("bank", bo)])
                if FILL:
                    P.add("pe", lambda e: e.matmul(bk(5, 0, 384), ident[:], uT[:, 0, 0:384], start=True, stop=True),
                          reads=[], writes=[("bank", 5)])
                if st["last"]:
                    if e_ == 0:
                        P.add("dve", lambda e: e.reciprocal(rec[0:64, :], bk(bo, 0, 512, p0=64, np_=64)),
                              reads=[("bank", bo)], writes=["rec"])
                        P.add("dve", lambda e: e.tensor_tensor(
                            xb(OTO + jj * 2048 + qc * 512, [[1, 512]], np_=64), bk(bo, 0, 512, np_=64),
                            rec[0:64, :], ALU.mult),
                            reads=[("bank", bo), "rec"], writes=[("OT", jj, 0, qc)])
                    else:
                        P.add("dve", lambda e: e.reciprocal(rec[64:128, :], bk(bo, 0, 512, np_=64)),
                              reads=[("bank", bo)], writes=["rec"])
                        P.add("dve", lambda e: e.tensor_tensor(
                            xb(OTO + jj * 2048 + qc * 512, [[1, 512]], p0=64, np_=64), bk(bo, 0, 512, p0=64, np_=64),
                            rec[64:128, :], ALU.mult),
                            reads=[("bank", bo), "rec"], writes=[("OT", jj, 1, qc)])

            inflight = []
            for st in steps:
                if st["need_mask"]:
                    emit_mask_rows()
                inflight.append(s_step(st))
                if len(inflight) > 2:
                    pv_step(*inflight.pop(0))
            while inflight:
                pv_step(*inflight.pop(0))

    def pool_mixer():
        P.add("pool", lambda e: e.memset(fscr[:, 1:2], 0.0), reads=[("OT", j, e_, qc) for j in range(4) for e_ in range(2) for qc in range(NTC)],
              writes=["Xep"])
        for off in (PB_F, TA_F, TB_F):
            P.add("pool", lambda e, off=off: e.memset(xf(off, [[1, 16]]), 0.0), writes=[("pl", "pad", off)])
        pkeys = [("pl", "p", tc) for tc in range(NTC)]

        def p_proj(g):
            sp_ = ring_use("win", g)
            for tc in range(NTC):
                bi = nbank()

                def mmp(e, sp_=sp_, tc=tc, bi=bi):
                    ins = None
                    for k in range(8):
                        ins = e.matmul(bk(bi, 0, 512), ring[:, sp_, k, :], uT[:, k, tc * 512:(tc + 1) * 512],
                                       start=(k == 0), stop=(k == 7))
                    return ins
                P.add("pe", mmp, reads=rk(sp_) + [("uT", 4 * tc + i) for i in range(4)], writes=[("bank", bi)])
                P.add("act", lambda e, tc=tc, bi=bi: e.activation(xf(PB_F + 16 + tc * 512, [[1, 512]]), bk(bi, 0, 512), AF.Copy),
                      reads=[("bank", bi), ("pl", "pad", PB_F)], writes=[("pl", "p", tc)])

        def chain(g):
            w = 2 ** (g + 1)
            src = PB_F
            dsts = [TA_F, TB_F]
            for i in range(g + 1):
                sh = 2 ** i
                dst = dsts[i % 2]
                P.add("dve", lambda e, src=src, dst=dst, sh=sh: e.tensor_tensor(
                    xf(dst + 16, [[1, S]]), xf(src + 16, [[1, S]]), xf(src + 16 - sh, [[1, S]]), ALU.add),
                    reads=pkeys + [("pl", "sum", src), ("pl", "pad", src)], writes=[("pl", "sum", dst), ("pl", "pad", dst)])
                src = dst
            oth = TA_F if src == TB_F else TB_F
            P.add("dve", lambda e, src=src, w=w: e.scalar_tensor_tensor(
                xb(MIXB, [[1, S]]), xf(src + 16, [[1, S]]), 1.0 / w, xf(PB_F + 16, [[1, S]]), ALU.mult, ALU.subtract),
                reads=pkeys + [("pl", "sum", src)], writes=[("pl", "mix")])
            P.add("pool", lambda e, src=src, g=g, oth=oth: e.tensor_tensor(
                xf(oth, [[1, 16]]), xf(src + 16, [[1, 16]]), invc[:, g, :], ALU.mult),
                reads=[("pl", "sum", src), "invc"], writes=[("pl", "pad", oth), ("pl", "fix")])
            P.add("pool", lambda e, oth=oth: e.tensor_tensor(
                xb(MIXB, [[1, 16]]), xf(oth, [[1, 16]]), xf(PB_F + 16, [[1, 16]]), ALU.subtract),
                reads=[("pl", "fix"), ("pl", "mix")] + pkeys, writes=[("pl", "mix2")])
            P.add("pool", lambda e, oth=oth: e.memset(xf(oth, [[1, 16]]), 0.0),
                  reads=[("pl", "mix2")], writes=[("pl", "pad", oth), ("pl", "fix")])

        def z_proj(g):
            for tc in range(NTC):
                bi = nbank()
                P.add("pe", lambda e, g=g, tc=tc, bi=bi: e.matmul(bk(bi, 0, 512), poolw[:, g, :],
                                                                  xb(MIXB + tc * 512, [[1, 512]]), start=True, stop=True),
                      reads=[("pl", "mix"), ("pl", "mix2"), "poolw"], writes=[("bank", bi)])
                P.add("act", lambda e, g=g, tc=tc, bi=bi: e.mul(
                    xb(ZTO + g * 2048 + tc * 512, [[1, 512]]), bk(bi, 0, 512), pscale[:, g:g + 1]),
                    reads=[("bank", bi), "pscale"], writes=[("zT", g, tc)])

        p_proj(0)
        for g in range(4):
            chain(g)
            if g + 1 < 4:
                p_proj(g + 1)
            z_proj(g)

    def merge_and_out():
        P.add("pool", lambda e: e.memset(fscr[:, 2:3], 0.0), reads=[("zT", g, tc) for g in range(4) for tc in range(NTC)],
              writes=["Xep"])
        for c in range(8):
            s0 = ring_use("win", 16 + c)
            s1 = ring_use("win", 24 + c)
            sb = ring_use("bpba", c)
            for tc in range(NTC):
                b0, b1, byp, bya = nbank("all"), nbank("all"), nbank("all"), nbank("all")
                ukeys = [("uT", 4 * tc + i) for i in range(4)]

                def mm0(e, s0=s0, tc=tc, b0=b0):
                    ins = None
                    for k in range(8):
                        ins = e.matmul(bk(b0, 0, 512), ring[:, s0, k, :], uT[:, k, tc * 512:(tc + 1) * 512],
                                       start=(k == 0), stop=(k == 7))
                    return ins

                def mm1(e, s1=s1, tc=tc, b1=b1):
                    ins = None
                    for k in range(8):
                        ins = e.matmul(bk(b1, 0, 512), ring[:, s1, k, :], uT[:, k, tc * 512:(tc + 1) * 512],
                                       start=(k == 0), stop=(k == 7))
                    return ins

                def mmyp(e, sb=sb, tc=tc, byp=byp):
                    ins = None
                    for g in range(4):
                        ins = e.matmul(bk(byp, 0, 512), ring[:, sb, g, :], xb(ZTO + g * 2048 + tc * 512, [[1, 512]]),
                                       start=(g == 0), stop=(g == 3))
                    return ins

                def mmya(e, sb=sb, tc=tc, bya=bya):
                    ins = None
                    for j in range(4):
                        ins = e.matmul(bk(bya, 0, 512), ring[:, sb, 4 + j, :], xb(OTO + j * 2048 + tc * 512, [[1, 512]]),
                                       start=(j == 0), stop=(j == 3))
                    return ins
                P.add("pe", mm0, reads=rk(s0) + ukeys, writes=[("bank", b0)])
                P.add("pe", mm1, reads=rk(s1) + ukeys, writes=[("bank", b1)])
                P.add("pe", mmyp, reads=rk(sb) + [("zT", g, tc) for g in range(4)], writes=[("bank", byp)])
                P.add("pe", mmya, reads=rk(sb) + [("OT", j, e_, tc) for j in range(4) for e_ in range(2)],
                      writes=[("bank", bya)])
                ta_, tb_ = 2 * (tc % 2), 2 * (tc % 2) + 1
                P.add("act", lambda e, b0=b0, ta_=ta_: e.activation(tmp[:, ta_, :], bk(b0, 0, 512), AF.Sigmoid),
                      reads=[("bank", b0)], writes=[("tmp", ta_)])
                P.add("act", lambda e, b1=b1, tb_=tb_: e.activation(tmp[:, tb_, :], bk(b1, 0, 512), AF.Sigmoid),
                      reads=[("bank", b1)], writes=[("tmp", tb_)])
                P.add("dve", lambda e, byp=byp, ta_=ta_: e.tensor_tensor(tmp[:, ta_, :], tmp[:, ta_, :], bk(byp, 0, 512), ALU.mult),
                      reads=[("tmp", ta_), ("bank", byp)], writes=[("tmp", ta_)])
                P.add("dve", lambda e, bya=bya, tb_=tb_: e.tensor_tensor(tmp[:, tb_, :], tmp[:, tb_, :], bk(bya, 0, 512), ALU.mult),
                      reads=[("tmp", tb_), ("bank", bya)], writes=[("tmp", tb_)])
                P.add("dve", lambda e, ta_=ta_, tb_=tb_, c=c, tc=tc: e.tensor_tensor(
                    xb(MRG + c * 2048 + tc * 512, [[1, 512]]), tmp[:, ta_, :], tmp[:, tb_, :], ALU.add),
                    reads=[("tmp", ta_), ("tmp", tb_)], writes=[("mrg", c, tc)])
        if debug:
            P.dma("pool", lambda e: e.dma_start(out=dbg_mrg, in_=xb(MRG, [[2048, 8], [1, 2048]])),
                  reads=[("mrg", c, tc) for c in range(8) for tc in range(NTC)], sem_key="dbg_mrg", is_out=True)
        wslots = [ring_use("wout", c, la=7 - c) for c in range(8)]
        for t in range(NT):
            for hf in range(2):
                bi = nbank("all")

                def mmo(e, t=t, hf=hf, bi=bi):
                    ins = None
                    for c in range(8):
                        ins = e.matmul(bk(bi, 0, 512), xb(MRG + c * 2048 + t * 128, [[1, 128]]),
                                       bass.AP(ring, wslots[c] * 1024 + hf * 512, [[8192, 128], [1, 512]]),
                                       start=(c == 0), stop=(c == 7))
                    return ins
                P.add("pe", mmo, reads=[("mrg", c, t // 4) for c in range(8)] + [k for c in range(8) for k in rk(wslots[c])],
                      writes=[("bank", bi)])
                P.add("dve", lambda e, t=t, hf=hf, bi=bi: e.tensor_tensor(
                    h[:, t, hf * 512:(hf + 1) * 512], bk(bi, 0, 512), h[:, t, hf * 512:(hf + 1) * 512], ALU.add),
                    reads=[("bank", bi), ("h", t, hf)], writes=[("h", t, hf)])
        ring_emit_upto(ring_state["used"] + LOOKAHEAD)

    def final_norm(s, next_s=None):
        fence()
        norm_stats(3)
        for t in range(NT):
            b = t % 8
            P.add("dve", lambda e, t=t, b=b: e.scalar_tensor_tensor(
                xf(OBUF_F + b * 1024, [[1, 1024]]), h[:, t, :], rstd[:, t:t + 1], gain[:], ALU.mult, ALU.mult),
                reads=[("h", t, 0), ("h", t, 1), "rstd", "gain"], writes=[("obuf", b)])
            P.dma("sp", lambda e, t=t, b=b: e.dma_start(out=out_d[s, t * 128:(t + 1) * 128, :],
                                                         in_=xf(OBUF_F + b * 1024, [[1, 1024]])),
                  reads=[("obuf", b)], sem_key=("out", b), is_out=True)
            if next_s is not None:
                load_x_tile(next_s, t)

    def dump(i):
        if not debug:
            return
        P.dma("sp", lambda e, i=i: e.dma_start(out=dbg_d[i].rearrange("(t p) d -> p t d", p=128), in_=h[:]),
              reads=[("h", t, hf) for t in range(NT) for hf in range(2)], sem_key=("dbg", i), is_out=True)

    for s in range(nseq):
        if s == 0:
            load_x(s)
        ffn("w1g", "w1u", w1d_d, norm_to_uT(0))
        dump(0)
        attention(norm_to_uT(1))
        if debug:
            P.dma("pool", lambda e: e.dma_start(out=dbg_ot, in_=xb(OTO, [[2048, 4], [1, 2048]])),
                  reads=[("OT", j, e_, qc) for j in range(4) for e_ in range(2) for qc in range(NTC)], sem_key="dbg_ot", is_out=True)
        pool_mixer()
        if debug:
            P.dma("pool", lambda e: e.dma_start(out=dbg_zt, in_=xb(ZTO, [[2048, 4], [1, 2048]])),
                  reads=[("zT", g, tc) for g in range(4) for tc in range(NTC)], sem_key="dbg_zt", is_out=True)
        merge_and_out()
        dump(1)
        ffn("w2g", "w2u", w2d_d, norm_to_uT(2))
        dump(2)
        final_norm(s, s + 1 if s + 1 < nseq else None)

    P.resolve()
    with contextlib.ExitStack() as st:
        sems = {}
        for i, sn in enumerate(P.sem_names()):
            sems[sn] = st.enter_context(nc.semaphore(f"s{i}"))
        P.emit(nc, sems)
    return nc


def _tile_cols(w):
    K, N = w.shape
    return np.ascontiguousarray(w.reshape(K // 128, 128, N // 128, 128).transpose(2, 1, 0, 3))


def _consts():
    f = np.float32
    ident = np.eye(128, dtype=f)
    i = np.arange(128)
    tri = np.where(i[:, None] > i[None, :], f(MBIG), f(0.0)).astype(f)
    t = np.arange(S)
    slopes = np.exp2(-np.arange(1, 9, dtype=np.float64)).astype(f)
    alibiq = np.zeros((8, 2, S), f)
    for hh in range(8):
        alibiq[hh, 0] = -8.0 * slopes[hh] * ((t // 64) * 64)
        alibiq[hh, 1] = -8.0 * slopes[hh] * (t % 64)
    kaaug = np.zeros((10, S), f)
    for n in range(8):
        kaaug[n] = (t // 256 == n)
    kaaug[8:] = 1.0
    expb = np.zeros((128, 8, 16), f)
    for hh in range(8):
        for kt in range(16):
            expb[:, hh, kt] = slopes[hh] * (kt * 128 + i)
    invc = np.zeros((128, 4, 16), f)
    for g in range(4):
        w = 2 ** (g + 1)
        invc[:, g, :] = 1.0 / np.minimum(np.arange(16) + 1, w)
    tri2 = np.concatenate([np.full((128, 128), MBIG, f), tri], axis=1)
    return dict(ident=ident, tri=tri, tri2=tri2, alibiq=alibiq, alibik=-alibiq, kaaug=kaaug, expb=expb, invc=invc)


_NC_CACHE = {}


def kernel(x, ffn1_norm, ffn1_w_gate, ffn1_w_up, ffn1_w_down, mix_norm, w_in,
           pool_w, pool_scale, w_branch_pool, w_branch_attn, w_out,
           ffn2_norm, ffn2_w_gate, ffn2_w_up, ffn2_w_down, final_norm):
    f = np.float32
    x = np.asarray(x, f)
    A = lambda a: np.asarray(a, f)
    shared = dict(
        w1g=_tile_cols(A(ffn1_w_gate)[0]), w1u=_tile_cols(A(ffn1_w_up)[0]),
        w2g=_tile_cols(A(ffn2_w_gate)[0]), w2u=_tile_cols(A(ffn2_w_up)[0]),
        win=_tile_cols(A(w_in)[0]),
        wbp=_tile_cols(A(w_branch_pool)[0]), wba=_tile_cols(A(w_branch_attn)[0]),
        w1d=np.ascontiguousarray(A(ffn1_w_down)[0]), w2d=np.ascontiguousarray(A(ffn2_w_down)[0]),
        wout=np.ascontiguousarray(A(w_out)[0]),
        poolw=np.ascontiguousarray(A(pool_w)[0].transpose(1, 0, 2)),
        gains=np.ascontiguousarray(np.broadcast_to(
            np.stack([A(ffn1_norm)[0], A(mix_norm)[0], A(ffn2_norm)[0], A(final_norm)])[:, None, :], (4, 128, D))),
        pscale=np.ascontiguousarray(A(pool_scale)[0].reshape(4, 128).T),
    )
    shared.update(_consts())
    if "nc" not in _NC_CACHE:
        _NC_CACHE["nc"] = build_program(NSEQ)
    nc = _NC_CACHE["nc"]
    in_maps = []
    for c in range(NCORES):
        m = dict(shared)
        m["x"] = np.ascontiguousarray(x[c * NSEQ:(c + 1) * NSEQ])
        in_maps.append(m)
    res = run_bass_kernel_spmd(nc, in_maps, core_ids=list(range(NCORES)))
    return np.concatenate([np.asarray(r["out"], f) for r in res.results], axis=0)
```

```python
import contextlib
import numpy as np
import concourse.bass as bass
import concourse.mybir as mybir
from concourse.bass_utils import run_bass_kernel_spmd

F32 = mybir.dt.float32
BF16 = mybir.dt.bfloat16
AF = mybir.ActivationFunctionType
ALU = mybir.AluOpType
AX = mybir.AxisListType

NCORES = 8
NSEQ = 4
S = 2048
D = 1024
DFF = 2816
NCH = DFF // 128
NT = S // 128
NTC = S // 512
EPS = 1e-6
MBIG = -240000.0
FILL = True
ENGS = ("pe", "act", "dve", "pool", "sp")
XNAMES = {"hT", "wd", "QA", "KA", "V", "PT", "OT", "zT", "gat", "pl", "mrg", "wout", "obuf"}


class Op:
    __slots__ = ("eng", "fn", "reads", "writes", "is_dma", "sem_key", "waits",
                 "signal", "need_signal", "idx", "is_out")

    def __init__(self, eng, fn, reads, writes, is_dma=False, sem_key=None, is_out=False):
        self.eng = eng
        self.fn = fn
        self.reads = tuple(reads)
        self.writes = tuple(writes)
        self.is_dma = is_dma
        self.sem_key = sem_key
        self.waits = []
        self.signal = None
        self.need_signal = False
        self.is_out = is_out


def _isx(k):
    return isinstance(k, tuple) and k[0] in XNAMES


class Prog:
    def __init__(self):
        self.ops = []
        self.final_waits = {}

    def _fix(self, reads, writes):
        reads = list(reads)
        if any(_isx(k) for k in reads) or any(_isx(k) for k in writes):
            reads.append("Xep")
        return reads, list(writes)

    def add(self, eng, fn, reads=(), writes=()):
        reads, writes = self._fix(reads, writes)
        op = Op(eng, fn, reads, writes)
        self.ops.append(op)
        return op

    def dma(self, queue, fn, reads=(), writes=(), sem_key=None, is_out=False):
        reads, writes = self._fix(reads, writes)
        op = Op(queue, fn, reads, writes, is_dma=True, sem_key=sem_key, is_out=is_out)
        self.ops.append(op)
        return op

    def resolve(self):
        last_writer = {}
        readers = {}
        deps_of = []
        for i, op in enumerate(self.ops):
            op.idx = i
            deps = set()
            for k in op.reads:
                w = last_writer.get(k)
                if w is not None:
                    deps.add(w)
            for k in op.writes:
                w = last_writer.get(k)
                if w is not None:
                    deps.add(w)
                for r in readers.get(k, ()):
                    deps.add(r)
            for k in op.reads:
                readers.setdefault(k, []).append(i)
            for k in op.writes:
                last_writer[k] = i
                readers[k] = []
            deps.discard(i)
            real = []
            for d in deps:
                dop = self.ops[d]
                if dop.is_dma:
                    real.append(d)
                    continue
                if dop.eng == op.eng:
                    if op.is_dma:
                        real.append(d)
                        continue
                    if op.eng == "pe":
                        continue
                    real.append(d)
                    continue
                real.append(d)
            deps_of.append(real)
            for d in real:
                self.ops[d].need_signal = True
        for op in self.ops:
            if op.is_dma and op.is_out:
                op.need_signal = True
        counters = {}
        for op in self.ops:
            if op.is_dma:
                sn = ("dma", op.sem_key)
                counters[sn] = counters.get(sn, 0) + 16
                op.signal = (sn, counters[sn])
            elif op.need_signal:
                sn = ("eng", op.eng)
                counters[sn] = counters.get(sn, 0) + 1
                op.signal = (sn, counters[sn])
        self.counters = counters
        waited = {e: {} for e in ENGS}
        for i, op in enumerate(self.ops):
            need = {}
            for d in deps_of[i]:
                sn, v = self.ops[d].signal
                if v > need.get(sn, 0):
                    need[sn] = v
            for sn, v in need.items():
                if waited[op.eng].get(sn, 0) >= v:
                    continue
                waited[op.eng][sn] = v
                op.waits.append((sn, v))
        for op in self.ops:
            if op.is_dma and op.is_out:
                sn, v = op.signal
                cur = self.final_waits.setdefault(op.eng, {})
                if v > cur.get(sn, 0):
                    cur[sn] = v
        return counters

    def sem_names(self):
        return sorted(self.counters.keys(), key=str)

    def emit(self, nc, sems):
        by_eng = {e: [] for e in ENGS}
        for op in self.ops:
            by_eng[op.eng].append(op)

        def run(engobj, ename):
            for op in by_eng[ename]:
                for sn, v in op.waits:
                    engobj.wait_ge(sems[sn], v)
                ins = op.fn(engobj)
                if op.signal is not None:
                    sn, v = op.signal
                    if op.is_dma:
                        ins.then_inc(sems[sn], 16)
                    elif op.need_signal:
                        ins.then_inc(sems[sn], 1)
            for sn, v in self.final_waits.get(ename, {}).items():
                engobj.wait_ge(sems[sn], v)

        with nc.Block() as block:
            @block.tensor
            def _(e):
                run(e, "pe")

            @block.scalar
            def _(e):
                run(e, "act")

            @block.vector
            def _(e):
                run(e, "dve")

            @block.gpsimd
            def _(e):
                run(e, "pool")

            @block.sync
            def _(e):
                run(e, "sp")


def build_program(nseq=NSEQ, debug=False):
    nc = bass.Bass("TRN2", target_bir_lowering=False)
    dbg_d = nc.dram_tensor("dbg", [4, S, D], F32, kind="ExternalOutput").ap() if debug else None
    dbg_ot = nc.dram_tensor("dbg_ot", [128, 4, S], F32, kind="ExternalOutput").ap() if debug else None
    dbg_zt = nc.dram_tensor("dbg_zt", [128, 4, S], F32, kind="ExternalOutput").ap() if debug else None
    dbg_mrg = nc.dram_tensor("dbg_mrg", [128, 8, S], F32, kind="ExternalOutput").ap() if debug else None

    def din(name, shape):
        return nc.dram_tensor(name, list(shape), F32, kind="ExternalInput").ap()

    x_d = din("x", [nseq, S, D])
    out_d = nc.dram_tensor("out", [nseq, S, D], F32, kind="ExternalOutput").ap()
    wgt = {
        "w1g": din("w1g", [NCH, 128, 8, 128]), "w1u": din("w1u", [NCH, 128, 8, 128]),
        "w2g": din("w2g", [NCH, 128, 8, 128]), "w2u": din("w2u", [NCH, 128, 8, 128]),
        "win": din("win", [32, 128, 8, 128]),
        "wbp": din("wbp", [8, 128, 4, 128]), "wba": din("wba", [8, 128, 4, 128]),
    }
    w1d_d = din("w1d", [DFF, D])
    w2d_d = din("w2d", [DFF, D])
    wout_d = din("wout", [D, D])
    poolw_d = din("poolw", [128, 4, 128])
    gains_d = din("gains", [4, 128, D])
    pscale_d = din("pscale", [128, 4])
    ident_d = din("ident", [128, 128])
    tri_d = din("tri", [128, 128])
    tri2_d = din("tri2", [128, 256])
    alibi_d = din("alibiq", [8, 2, S])
    alibik_d = din("alibik", [8, 2, S])
    kaaug_d = din("kaaug", [10, S])
    expb_d = din("expb", [128, 8, 16])
    invc_d = din("invc", [128, 4, 16])

    h = nc.alloc_sbuf_tensor("h", [128, NT, D], F32)
    uT = nc.alloc_sbuf_tensor("uT", [128, 8, S], BF16)
    ring = nc.alloc_sbuf_tensor("ring", [128, 8, 8, 128], BF16)
    gain = nc.alloc_sbuf_tensor("gain", [128, D], F32)
    ub = nc.alloc_sbuf_tensor("ub", [128, 2, D], BF16)
    junk = nc.alloc_sbuf_tensor("junk", [128, D], BF16)
    tmp = nc.alloc_sbuf_tensor("tmp", [128, 4, 512], F32)
    ss = nc.alloc_sbuf_tensor("ss", [128, NT], F32)
    rstd = nc.alloc_sbuf_tensor("rstd", [128, NT], F32)
    ident = nc.alloc_sbuf_tensor("ident_s", [128, 128], BF16)
    tri = nc.alloc_sbuf_tensor("tri_s", [128, 128], BF16)
    tri2 = nc.alloc_sbuf_tensor("tri2_s", [128, 256], BF16)
    expb = nc.alloc_sbuf_tensor("expb_s", [128, 8, 16], F32)
    invc = nc.alloc_sbuf_tensor("invc_s", [128, 4, 16], F32)
    pscale = nc.alloc_sbuf_tensor("pscale_s", [128, 4], F32)
    poolw = nc.alloc_sbuf_tensor("poolw_s", [128, 4, 128], BF16)
    fscr = nc.alloc_sbuf_tensor("fscr", [128, 8], F32)
    XR = 32768
    XB = nc.alloc_sbuf_tensor("X", [128, XR], BF16)
    XF = XB.bitcast(F32)
    XRF = XR // 2
    ps = nc.alloc_psum_tensor("ps", [128, 4096], F32)
    psb = ps.bitcast(BF16)
    rec = nc.alloc_sbuf_tensor("rec", [128, 512], F32)
    V2 = nc.alloc_sbuf_tensor("V2", [128, 3072], BF16)

    def xb(off, dims, p0=0, np_=128):
        return bass.AP(XB, p0 * XR + off, [[XR, np_]] + [list(d) for d in dims])

    def xf(off, dims, p0=0, np_=128):
        return bass.AP(XF, p0 * XRF + off, [[XRF, np_]] + [list(d) for d in dims])

    def bk(i, c0, n, p0=0, np_=128):
        return bass.AP(ps, p0 * 4096 + i * 512 + c0, [[4096, np_], [1, n]])

    HT = lambda b: b * 8192
    WD = lambda b: 16384 + b * 4096
    QA = lambda e: e * 2048
    KA = lambda e: 4096 + e * 2048
    VO = 8192
    PT = lambda i: 11264 + i * 1024
    GCP_F = 7168
    CMP_F = 7296
    RANK_F = 7424
    MBP = 14880
    KMS_F = 8016
    KMB = 16064
    OTO = 16384
    ZTO = 24576
    PB_F, TA_F, TB_F = 0, 2064, 4128
    MIXB = 12384
    MRG = 0
    WOUT = 16384
    OBUF_F = 0

    P = Prog()
    att_state = {"sp": 0}
    pools = {"main": [0, 1, 2, 3, 4, 5], "acc": [6, 7], "all": [0, 1, 2, 3, 4, 5, 6, 7], "aux": [4, 5]}
    pool_ctr = {"main": 0, "acc": 0, "all": 0, "aux": 0}

    def nbank(pool="main"):
        lst = pools[pool]
        b = lst[pool_ctr[pool] % len(lst)]
        pool_ctr[pool] += 1
        return b

    def fence():
        P.add("pool", lambda e: e.memset(fscr[:, 0:1], 0.0), reads=[], writes=["Xep"])

    uses = []
    for s in range(nseq):
        for c in range(NCH):
            uses += [("w1g", c), ("w1u", c)]
        for j in range(4):
            uses += [("win", 8 + j), ("win", 12 + j), ("win", 4 + j)]
        for g in range(4):
            uses += [("win", g)]
        for c in range(8):
            uses += [("win", 16 + c), ("win", 24 + c), ("bpba", c)]
        for c in range(8):
            uses += [("wout", c)]
        for c in range(NCH):
            uses += [("w2g", c), ("w2u", c)]
    ring_state = {"emitted": 0, "used": 0}
    LOOKAHEAD = 5

    def ring_emit_upto(n):
        while ring_state["emitted"] < min(n, len(uses)):
            m = ring_state["emitted"]
            kind, idx = uses[m]
            slot = m % 8
            if kind == "bpba":
                P.dma("pool", lambda e, slot=slot, idx=idx: e.dma_start(out=ring[:, slot, 0:4, :], in_=wgt["wbp"][idx]),
                      writes=[("ring", slot)], sem_key=("ring", slot, 0))
                P.dma("pool", lambda e, slot=slot, idx=idx: e.dma_start(out=ring[:, slot, 4:8, :], in_=wgt["wba"][idx]),
                      writes=[("ringb", slot)], reads=[("ring", slot)], sem_key=("ring", slot, 1))
            elif kind == "wout":
                P.dma("pool", lambda e, slot=slot, idx=idx: e.dma_start(
                    out=bass.AP(ring, slot * 1024, [[8192, 128], [1, 1024]]), in_=wout_d[idx * 128:(idx + 1) * 128, :]),
                    writes=[("ring", slot), ("ringb", slot)], sem_key=("ring", slot, 0))
            else:
                P.dma("pool", lambda e, slot=slot, kind=kind, idx=idx: e.dma_start(out=ring[:, slot, :, :], in_=wgt[kind][idx]),
                      writes=[("ring", slot), ("ringb", slot)], sem_key=("ring", slot, 0))
            ring_state["emitted"] += 1

    def ring_use(kind, idx, la=LOOKAHEAD):
        m = ring_state["used"]
        assert uses[m] == (kind, idx), (uses[m], kind, idx)
        ring_emit_upto(m + 1 + la)
        ring_state["used"] += 1
        return m % 8

    def rk(slot):
        return [("ring", slot), ("ringb", slot)]

    P.dma("pool", lambda e: e.dma_start(out=ident[:], in_=ident_d), writes=["ident"], sem_key="c_ident")
    P.dma("pool", lambda e: e.dma_start(out=tri[:], in_=tri_d), writes=["tri"], sem_key="c_tri")
    P.dma("pool", lambda e: e.dma_start(out=tri2[:], in_=tri2_d), writes=["tri2"], sem_key="c_tri2")
    P.dma("pool", lambda e: e.dma_start(out=poolw[:], in_=poolw_d), writes=["poolw"], sem_key="c_poolw")
    P.dma("sp", lambda e: e.dma_start(out=expb[:], in_=expb_d), writes=["expb"], sem_key="c_expb")
    P.dma("sp", lambda e: e.dma_start(out=invc[:], in_=invc_d), writes=["invc"], sem_key="c_invc")
    P.dma("sp", lambda e: e.dma_start(out=pscale[:], in_=pscale_d), writes=["pscale"], sem_key="c_pscale")

    def load_x_tile(s, t):
        P.dma("sp", lambda e, t=t, s=s: e.dma_start(out=h[:, t, :], in_=x_d[s, t * 128:(t + 1) * 128, :]),
              writes=[("h", t, 0), ("h", t, 1)], sem_key=("x", t))

    def load_x(s):
        for t in range(NT):
            load_x_tile(s, t)

    def norm_stats(gidx):
        P.dma("sp", lambda e: e.dma_start(out=gain[:], in_=gains_d[gidx]), writes=["gain"], sem_key="gain")
        P.add("act", lambda e: e.memzero(ss[:]), writes=[("ss", t) for t in range(NT)])
        for t in range(NT):
            P.add("act", lambda e, t=t: e.activation(junk[:], h[:, t, :], AF.Square, accum_out=ss[:, t:t + 1]),
                  reads=[("h", t, 0), ("h", t, 1), ("ss", t)], writes=["junk", ("ss", t)])
        allss = [("ss", t) for t in range(NT)]
        P.add("dve", lambda e: e.tensor_scalar(rstd[:], ss[:], 1.0 / D, EPS, ALU.mult, ALU.add),
              reads=allss, writes=["rstd"])
        P.add("act", lambda e: e.activation(rstd[:], rstd[:], AF.Sqrt), reads=["rstd"], writes=["rstd"])
        P.add("dve", lambda e: e.reciprocal(rstd[:], rstd[:]), reads=["rstd"], writes=["rstd"])

    def norm_to_uT(gidx):
        norm_stats(gidx)

        def emit_tiles(t0):
            for t in range(t0, t0 + 4):
                b = t % 2
                P.add("dve", lambda e, t=t, b=b: e.scalar_tensor_tensor(ub[:, b, :], h[:, t, :], rstd[:, t:t + 1], gain[:],
                                                                        ALU.mult, ALU.mult),
                      reads=[("h", t, 0), ("h", t, 1), "rstd", "gain"], writes=[("ub", b)])
                bi = nbank("all")

                def tr(e, b=b, bi=bi):
                    ins = None
                    for k in range(8):
                        ins = e.transpose(bass.AP(psb, bi * 1024 + k * 128, [[8192, 128], [1, 128]]),
                                          ub[:, b, k * 128:(k + 1) * 128], ident[:])
                    return ins
                P.add("pe", tr, reads=[("ub", b), "ident"], writes=[("bank", bi)])
                P.add("act", lambda e, t=t, bi=bi: e.activation(
                    bass.AP(uT, t * 128, [[8 * S, 128], [S, 8], [1, 128]]),
                    bass.AP(psb, bi * 1024, [[8192, 128], [128, 8], [1, 128]]), AF.Copy),
                    reads=[("bank", bi)], writes=[("uT", t)])
        return [lambda t0=t0: emit_tiles(t0) for t0 in (0, 4, 8, 12)]

    def ffn(gk, uk, wd_d, pending):
        parts = [(0, 4), (4, 8), (8, 12), (12, 16), (16, 20), (20, 22)]
        fence()

        def gu(pi):
            c0, c1 = parts[pi]
            hb = pi % 2
            P.dma("pool", lambda e, c0=c0, c1=c1, hb=hb: e.dma_start(
                out=xb(WD(hb), [[1024, c1 - c0], [1, 1024]]),
                in_=wd_d[c0 * 128:c1 * 128, :].rearrange("(c p) n -> p c n", p=128)),
                writes=[("wd", hb)], sem_key=("wd", hb))
            for c in range(c0, c1):
                sg = ring_use(gk, c)
                su = ring_use(uk, c)
                for tc in range(NTC):
                    if pending and tc == 0:
                        pending.pop(0)()
                    if pending:
                        pending.pop(0)()
                    bg = nbank("all")
                    bu = nbank("all")

                    def mm(e, sg=sg, su=su, tc=tc, bg=bg, bu=bu):
                        ins = None
                        for k in range(8):
                            ins = e.matmul(bk(bg, 0, 512), ring[:, sg, k, :], uT[:, k, tc * 512:(tc + 1) * 512],
                                           start=(k == 0), stop=(k == 7))
                        for k in range(8):
                            ins = e.matmul(bk(bu, 0, 512), ring[:, su, k, :], uT[:, k, tc * 512:(tc + 1) * 512],
                                           start=(k == 0), stop=(k == 7))
                        return ins
                    P.add("pe", mm, reads=rk(sg) + rk(su) + [("uT", 4 * tc + i) for i in range(4)],
                          writes=[("bank", bg), ("bank", bu)])
                    tb_ = (c * NTC + tc) % 4
                    P.add("act", lambda e, bg=bg, tb_=tb_: e.activation(tmp[:, tb_, :], bk(bg, 0, 512), AF.Silu),
                          reads=[("bank", bg)], writes=[("tmp", tb_)])
                    P.add("dve", lambda e, bu=bu, tb_=tb_, hb=hb, cc=c - c0, tc=tc: e.tensor_tensor(
                        xb(HT(hb) + cc * 2048 + tc * 512, [[1, 512]]), tmp[:, tb_, :], bk(bu, 0, 512), ALU.mult),
                        reads=[("tmp", tb_), ("bank", bu)], writes=[("hT", hb, c - c0, tc)])

        def down(pi):
            c0, c1 = parts[pi]
            hb = pi % 2
            n = c1 - c0
            for t in range(NT):
                for hf in range(2):
                    bi = nbank("all")

                    def mm(e, t=t, hf=hf, bi=bi, n=n, hb=hb):
                        ins = None
                        for cc in range(n):
                            ins = e.matmul(bk(bi, 0, 512), xb(HT(hb) + cc * 2048 + t * 128, [[1, 128]]),
                                           xb(WD(hb) + cc * 1024 + hf * 512, [[1, 512]]),
                                           start=(cc == 0), stop=(cc == n - 1))
                        return ins
                    P.add("pe", mm, reads=[("hT", hb, cc, t // 4) for cc in range(n)] + [("wd", hb)],
                          writes=[("bank", bi)])
                    P.add("dve", lambda e, t=t, hf=hf, bi=bi: e.scalar_tensor_tensor(
                        h[:, t, hf * 512:(hf + 1) * 512], bk(bi, 0, 512), 0.5, h[:, t, hf * 512:(hf + 1) * 512],
                        ALU.mult, ALU.add),
                        reads=[("bank", bi), ("h", t, hf)], writes=[("h", t, hf)])

        gu(0)
        for pi in range(1, len(parts)):
            gu(pi)
            down(pi - 1)
        down(len(parts) - 1)

    def attention(pending):
        fence()
        QAo = lambda e_, par: (e_ * 2048) if par == 0 else (ZTO + e_ * 2048)
        KAo = lambda e_, par: (4096 + e_ * 2048) if par == 0 else (ZTO + 4096 + e_ * 2048)

        def vap(par, off, dims, p0=0, np_=128):
            if par == 0:
                return xb(VO + off, dims, p0=p0, np_=np_)
            return bass.AP(V2, p0 * 3072 + off, [[3072, np_]] + [list(d) for d in dims])

        for par in range(2):
            for e_ in range(2):
                P.dma("pool", lambda e, e_=e_, par=par: e.dma_start(out=xb(KAo(e_, par), [[1, S]], p0=64, np_=10), in_=kaaug_d),
                      writes=[("KA", par, e_, "aug")], sem_key=("kaaug", par, e_))
                P.dma("pool", lambda e, e_=e_, par=par: e.dma_start(out=xb(QAo(e_, par), [[1, S]], p0=74, np_=2), in_=kaaug_d[8:10, :]),
                      writes=[("QA", par, e_, "ones")], sem_key=("qaones", par, e_))
            P.add("pool", lambda e, par=par: e.memset(vap(par, 0, [[1, 16 * 192]]), 1.0), writes=[("V", par, "all")])
        P.add("pool", lambda e: e.memset(xb(MBP, [[1, 8 * 2 * 72]]), 0.0), writes=[("gat", "mbp")])

        def proj_units(j):
            par = j % 2
            stt = {}
            units = []

            def uA():
                for e_ in range(2):
                    hh = 2 * j + e_
                    P.add("pool", lambda e, e_=e_: e.memset(xb(QAo(e_, par), [[1, S]], p0=64, np_=8), 0.0),
                          writes=[("QA", par, e_, "mask")])
                    P.dma("pool", lambda e, e_=e_, hh=hh: e.dma_start(out=xb(QAo(e_, par), [[1, S]], p0=72, np_=2), in_=alibi_d[hh]),
                          writes=[("QA", par, e_, "alibi")], sem_key=("alibi", par, e_))
                    P.dma("pool", lambda e, e_=e_, hh=hh: e.dma_start(out=xb(KAo(e_, par), [[1, S]], p0=74, np_=2), in_=alibik_d[hh]),
                          writes=[("KA", par, e_, "kpos")], sem_key=("alibik", par, e_))
            units.append(uA)

            def uB(tc):
                if tc == 0:
                    stt["sk"] = ring_use("win", 8 + j)
                sk = stt["sk"]
                if pending and tc == 0:
                    pending.pop(0)()
                if pending:
                    pending.pop(0)()
                bi = nbank("aux")

                def mmk(e):
                    ins = None
                    for k in range(8):
                        ins = e.matmul(bk(bi, 0, 512), ring[:, sk, k, :], uT[:, k, tc * 512:(tc + 1) * 512],
                                       start=(k == 0), stop=(k == 7))
                    return ins
                P.add("pe", mmk, reads=rk(sk) + [("uT", 4 * tc + i) for i in range(4)], writes=[("bank", bi)])
                for e_ in range(2):
                    P.add("dve", lambda e, e_=e_: e.tensor_copy(
                        xb(KAo(e_, par) + tc * 512, [[1, 512]], np_=64), bk(bi, 0, 512, p0=64 * e_, np_=64)),
                        reads=[("bank", bi)], writes=[("KA", par, e_, tc)])
                    P.add("dve", lambda e, e_=e_: e.tensor_reduce(
                        xf(KMS_F + e_ * 8 + 2 * tc, [[1, 2]], np_=64),
                        bass.AP(ps, e_ * 64 * 4096 + bi * 512, [[4096, 64], [256, 2], [1, 256]]), AX.X, ALU.add),
                        reads=[("bank", bi)], writes=[("gat", "kms", e_, tc)])
            units += [(lambda tc=tc: uB(tc)) for tc in range(NTC)]

            def uKmb():
                P.add("dve", lambda e: e.tensor_scalar(xb(KMB, [[1, 16]], np_=64), xf(KMS_F, [[1, 16]], np_=64),
                                                       1.0 / 256, 0.0, ALU.mult, ALU.add),
                      reads=[("gat", "kms", e_, tc) for e_ in range(2) for tc in range(NTC)], writes=[("gat", "kmb")])
            units.append(uKmb)

            def uC(t4):
                if t4 == 0:
                    stt["sv"] = ring_use("win", 12 + j)
                sv = stt["sv"]
                bi = nbank("aux")

                def mmv(e):
                    ins = None
                    for tt in range(4):
                        t = t4 * 4 + tt
                        for k in range(8):
                            ins = e.matmul(bk(bi, tt * 128, 128), uT[:, k, t * 128:(t + 1) * 128], ring[:, sv, k, :],
                                           start=(k == 0), stop=(k == 7))
                    return ins
                P.add("pe", mmv, reads=rk(sv) + [("uT", t4 * 4 + i) for i in range(4)], writes=[("bank", bi)])
                P.add("dve", lambda e: e.tensor_copy(
                    vap(par, t4 * 4 * 192, [[192, 4], [128, 2], [1, 64]]),
                    bass.AP(ps, bi * 512, [[4096, 128], [128, 4], [64, 2], [1, 64]])),
                    reads=[("bank", bi), ("V", par, "all")], writes=[("V", par, t4)])
            units += [(lambda t4=t4: uC(t4)) for t4 in range(4)]

            def uD(tc):
                if tc == 0:
                    stt["sq"] = ring_use("win", 4 + j)
                sq = stt["sq"]
                bi = nbank("aux")

                def mmq(e):
                    ins = None
                    for k in range(8):
                        ins = e.matmul(bk(bi, 0, 512), ring[:, sq, k, :], uT[:, k, tc * 512:(tc + 1) * 512],
                                       start=(k == 0), stop=(k == 7))
                    return ins
                P.add("pe", mmq, reads=rk(sq) + [("uT", 4 * tc + i) for i in range(4)], writes=[("bank", bi)])
                for e_ in range(2):
                    P.add("dve", lambda e, e_=e_: e.tensor_copy(
                        xb(QAo(e_, par) + tc * 512, [[1, 512]], np_=64), bk(bi, 0, 512, p0=64 * e_, np_=64)),
                        reads=[("bank", bi)], writes=[("QA", par, e_, tc)])
            units += [(lambda tc=tc: uD(tc)) for tc in range(NTC)]

            def uE():
                bg = nbank("aux")

                def mmg(e):
                    ins = None
                    for t8 in range(8):
                        t = 8 + t8
                        for e_ in range(2):
                            ins = e.matmul(bk(bg, (t8 * 2 + e_) * 8, 8), xb(QAo(e_, par) + t * 128, [[1, 128]], np_=64),
                                           xb(KMB + e_ * 8, [[1, 8]], np_=64), start=True, stop=True)
                    return ins
                P.add("pe", mmg, reads=[("QA", par, e_, tc) for e_ in range(2) for tc in (2, 3)] + [("gat", "kmb")],
                      writes=[("bank", bg)])
                P.add("dve", lambda e: e.tensor_copy(xf(GCP_F, [[1, 128]]), bk(bg, 0, 128)),
                      reads=[("bank", bg)], writes=[("gat", "gcp")])
                for t8 in range(8):
                    n = (8 + t8) // 2
                    gj = xf(GCP_F + t8 * 16, [[8, 2], [0, n], [1, n]])
                    gi = xf(GCP_F + t8 * 16, [[8, 2], [1, n], [0, n]])
                    P.add("dve", lambda e, gj=gj, gi=gi, n=n: e.tensor_tensor(xf(CMP_F, [[n * n, 2], [n, n], [1, n]]), gj, gi, ALU.is_gt),
                          reads=[("gat", "gcp")], writes=[("gat", "cmp")])
                    P.add("dve", lambda e, n=n: e.tensor_reduce(xf(RANK_F, [[8, 2], [1, n]]),
                                                                xf(CMP_F, [[n * n, 2], [n, n], [1, n]]), AX.X, ALU.add),
                          reads=[("gat", "cmp")], writes=[("gat", "rank")])
                    P.add("dve", lambda e, n=n, t8=t8: e.tensor_scalar(
                        xb(MBP + t8 * 144 + 64, [[72, 2], [1, n]]), xf(RANK_F, [[8, 2], [1, n]]), 2.5, MBIG, ALU.is_ge, ALU.mult),
                        reads=[("gat", "rank"), ("gat", "mbp")], writes=[("gat", "mb", t8)])
            units.append(uE)
            return units

        def core(j, units):
            par = j % 2

            def emit_mask_rows():
                for e_ in range(2):
                    for hf in range(2):
                        bi = nbank("aux")

                        def mmt(e, e_=e_, hf=hf, bi=bi):
                            ins = None
                            for tt in range(4):
                                t8 = hf * 4 + tt
                                ins = e.matmul(bk(bi, tt * 128, 128, np_=72), xb(MBP + t8 * 144 + e_ * 72, [[1, 72]]),
                                               ident[:], start=True, stop=True)
                            return ins
                        P.add("pe", mmt, reads=[("gat", "mb", hf * 4 + tt) for tt in range(4)] + ["ident"],
                              writes=[("bank", bi)])
                        P.add("dve", lambda e, e_=e_, hf=hf, bi=bi: e.tensor_copy(
                            xb(QAo(e_, par) + 1024 + hf * 512, [[1, 512]], p0=64, np_=8), bk(bi, 0, 512, p0=64, np_=8)),
                            reads=[("bank", bi), ("QA", par, e_, "mask")], writes=[("QA", par, e_, "mask2", hf)])

            steps = []
            for gi_, (e_, qc) in enumerate([(0, 0), (0, 1), (1, 0), (1, 1), (0, 2), (0, 3), (1, 2), (1, 3)]):
                bo = nbank("acc")
                nkt = 4 * qc + 4
                for p in range(nkt // 2):
                    steps.append(dict(e_=e_, qc=qc, p=p, bo=bo, nkt=nkt, last=(p == nkt // 2 - 1),
                                      need_mask=(gi_ == 4 and p == 0)))

            def s_step(st):
                e_, qc, p = st["e_"], st["qc"], st["p"]
                qa_keys = [("QA", par, e_, "mask"), ("QA", par, e_, "alibi"), ("QA", par, e_, "ones")]
                if qc >= 2:
                    qa_keys += [("QA", par, e_, "mask2", qc - 2)]
                ka_keys = [("KA", par, e_, "aug"), ("KA", par, e_, "kpos")]
                sp = att_state["sp"] % 2
                pp = att_state["sp"] % 3
                att_state["sp"] += 1
                kt0 = 2 * p
                r0 = kt0 - 4 * qc
                c0 = 128 * r0 if r0 > 0 else 0

                def mms(e):
                    ins = None
                    for i in range(2):
                        kt = kt0 + i
                        r = kt - 4 * qc
                        b = 2 * sp + i
                        ins = e.matmul(bk(b, c0, 512 - c0), xb(KAo(e_, par) + kt * 128, [[1, 128]], np_=76),
                                       xb(QAo(e_, par) + qc * 512 + c0, [[1, 512 - c0]], np_=76),
                                       start=True, stop=(r < 0))
                        if r >= 0 and i == 0:
                            ins = e.matmul(bk(b, 128 * r, 128), ident[:], tri[:], start=False, stop=True)
                        elif r >= 0:
                            ins = e.matmul(bk(b, c0, 256), ident[:], tri2[:], start=False, stop=True)
                    return ins
                P.add("pe", mms, reads=[("KA", par, e_, kt0 // 4), ("QA", par, e_, qc), "ident", "tri", "tri2"] + qa_keys + ka_keys,
                      writes=[("bank", 2 * sp), ("bank", 2 * sp + 1)])
                P.add("act", lambda e: e.activation(
                    xb(PT(pp) + c0, [[512, 2], [1, 512 - c0]]),
                    bass.AP(ps, 2 * sp * 512 + c0, [[4096, 128], [512, 2], [1, 512 - c0]]), AF.Exp, scale=0.125),
                    reads=[("bank", 2 * sp), ("bank", 2 * sp + 1)], writes=[("PT", pp)])
                return (st, kt0, pp)

            def pv_step(st, kt0, pp):
                e_, qc, bo, nkt = st["e_"], st["qc"], st["bo"], st["nkt"]

                def mmpv(e):
                    ins = None
                    for i in range(2):
                        kt = kt0 + i
                        r = kt - 4 * qc
                        c0i = 128 * r if r > 0 else 0
                        ins = e.matmul(bk(bo, c0i, 512 - c0i), vap(par, kt * 192 + e_ * 64, [[1, 128]]),
                                       xb(PT(pp) + i * 512 + c0i, [[1, 512 - c0i]]),
                                       start=(kt == 0), stop=(kt == nkt - 1))
                    return ins
                P.add("pe", mmpv, reads=[("PT", pp), ("V", par, kt0 // 4), ("V", par, "all")], writes=[("bank", bo)])
                if st["last"]:
                    lo = 64 * e_
                    hi = 64 * (1 - e_)
                    P.add("dve", lambda e: e.reciprocal(rec[lo:lo + 64, :], bk(bo, 0, 512, p0=hi, np_=64)),
                          reads=[("bank", bo)], writes=["rec"])
                    P.add("dve", lambda e: e.tensor_tensor(
                        xb(OTO + j * 2048 + qc * 512, [[1, 512]], p0=lo, np_=64), bk(bo, 0, 512, p0=lo, np_=64),
                        rec[lo:lo + 64, :], ALU.mult),
                        reads=[("bank", bo), "rec"], writes=[("OT", j, e_, qc)])

            inflight = []
            for idx, st in enumerate(steps):
                if st["need_mask"]:
                    emit_mask_rows()
                inflight.append(s_step(st))
                if len(inflight) > 2:
                    pv_step(*inflight.pop(0))
                    if units and idx >= 2 and idx % 2 == 0:
                        units.pop(0)()
                    elif FILL and not units:
                        P.add("pe", lambda e: e.matmul(bk(5, 0, 384), ident[:], uT[:, 0, 0:384], start=True, stop=True),
                              reads=[], writes=[("bank", 5)])
            while inflight:
                pv_step(*inflight.pop(0))
            while units:
                units.pop(0)()

        for u in proj_units(0):
            u()
        for j in range(4):
            core(j, proj_units(j + 1) if j < 3 else [])

    def pool_mixer():
        P.add("pool", lambda e: e.memset(fscr[:, 1:2], 0.0), reads=[("OT", j, e_, qc) for j in range(4) for e_ in range(2) for qc in range(NTC)],
              writes=["Xep"])
        for off in (PB_F, TA_F, TB_F):
            P.add("pool", lambda e, off=off: e.memset(xf(off, [[1, 16]]), 0.0), writes=[("pl", "pad", off)])
        pkeys = [("pl", "p", tc) for tc in range(NTC)]

        def p_proj(g):
            sp_ = ring_use("win", g)
            for tc in range(NTC):
                bi = nbank()

                def mmp(e, sp_=sp_, tc=tc, bi=bi):
                    ins = None
                    for k in range(8):
                        ins = e.matmul(bk(bi, 0, 512), ring[:, sp_, k, :], uT[:, k, tc * 512:(tc + 1) * 512],
                                       start=(k == 0), stop=(k == 7))
                    return ins
                P.add("pe", mmp, reads=rk(sp_) + [("uT", 4 * tc + i) for i in range(4)], writes=[("bank", bi)])
                P.add("act", lambda e, tc=tc, bi=bi: e.activation(xf(PB_F + 16 + tc * 512, [[1, 512]]), bk(bi, 0, 512), AF.Copy),
                      reads=[("bank", bi), ("pl", "pad", PB_F)], writes=[("pl", "p", tc)])

        def chain(g):
            w = 2 ** (g + 1)
            src = PB_F
            dsts = [TA_F, TB_F]
            for i in range(g + 1):
                sh = 2 ** i
                dst = dsts[i % 2]
                P.add("dve", lambda e, src=src, dst=dst, sh=sh: e.tensor_tensor(
                    xf(dst + 16, [[1, S]]), xf(src + 16, [[1, S]]), xf(src + 16 - sh, [[1, S]]), ALU.add),
                    reads=pkeys + [("pl", "sum", src), ("pl", "pad", src)], writes=[("pl", "sum", dst), ("pl", "pad", dst)])
                src = dst
            oth = TA_F if src == TB_F else TB_F
            P.add("dve", lambda e, src=src, w=w: e.scalar_tensor_tensor(
                xb(MIXB, [[1, S]]), xf(src + 16, [[1, S]]), 1.0 / w, xf(PB_F + 16, [[1, S]]), ALU.mult, ALU.subtract),
                reads=pkeys + [("pl", "sum", src)], writes=[("pl", "mix")])
            P.add("pool", lambda e, src=src, g=g, oth=oth: e.tensor_tensor(
                xf(oth, [[1, 16]]), xf(src + 16, [[1, 16]]), invc[:, g, :], ALU.mult),
                reads=[("pl", "sum", src), "invc"], writes=[("pl", "pad", oth), ("pl", "fix")])
            P.add("pool", lambda e, oth=oth: e.tensor_tensor(
                xb(MIXB, [[1, 16]]), xf(oth, [[1, 16]]), xf(PB_F + 16, [[1, 16]]), ALU.subtract),
                reads=[("pl", "fix"), ("pl", "mix")] + pkeys, writes=[("pl", "mix2")])
            P.add("pool", lambda e, oth=oth: e.memset(xf(oth, [[1, 16]]), 0.0),
                  reads=[("pl", "mix2")], writes=[("pl", "pad", oth), ("pl", "fix")])

        def z_proj(g):
            for tc in range(NTC):
                bi = nbank()
                P.add("pe", lambda e, g=g, tc=tc, bi=bi: e.matmul(bk(bi, 0, 512), poolw[:, g, :],
                                                                  xb(MIXB + tc * 512, [[1, 512]]), start=True, stop=True),
                      reads=[("pl", "mix"), ("pl", "mix2"), "poolw"], writes=[("bank", bi)])
                P.add("act", lambda e, g=g, tc=tc, bi=bi: e.mul(
                    xb(ZTO + g * 2048 + tc * 512, [[1, 512]]), bk(bi, 0, 512), pscale[:, g:g + 1]),
                    reads=[("bank", bi), "pscale"], writes=[("zT", g, tc)])

        p_proj(0)
        for g in range(4):
            chain(g)
            if g + 1 < 4:
                p_proj(g + 1)
            z_proj(g)

    def merge_and_out():
        P.add("pool", lambda e: e.memset(fscr[:, 2:3], 0.0), reads=[("zT", g, tc) for g in range(4) for tc in range(NTC)],
              writes=["Xep"])
        for c in range(8):
            s0 = ring_use("win", 16 + c)
            s1 = ring_use("win", 24 + c)
            sb = ring_use("bpba", c)
            for tc in range(NTC):
                b0, b1, byp, bya = nbank("all"), nbank("all"), nbank("all"), nbank("all")
                ukeys = [("uT", 4 * tc + i) for i in range(4)]

                def mm0(e, s0=s0, tc=tc, b0=b0):
                    ins = None
                    for k in range(8):
                        ins = e.matmul(bk(b0, 0, 512), ring[:, s0, k, :], uT[:, k, tc * 512:(tc + 1) * 512],
                                       start=(k == 0), stop=(k == 7))
                    return ins

                def mm1(e, s1=s1, tc=tc, b1=b1):
                    ins = None
                    for k in range(8):
                        ins = e.matmul(bk(b1, 0, 512), ring[:, s1, k, :], uT[:, k, tc * 512:(tc + 1) * 512],
                                       start=(k == 0), stop=(k == 7))
                    return ins

                def mmyp(e, sb=sb, tc=tc, byp=byp):
                    ins = None
                    for g in range(4):
                        ins = e.matmul(bk(byp, 0, 512), ring[:, sb, g, :], xb(ZTO + g * 2048 + tc * 512, [[1, 512]]),
                                       start=(g == 0), stop=(g == 3))
                    return ins

                def mmya(e, sb=sb, tc=tc, bya=bya):
                    ins = None
                    for j in range(4):
                        ins = e.matmul(bk(bya, 0, 512), ring[:, sb, 4 + j, :], xb(OTO + j * 2048 + tc * 512, [[1, 512]]),
                                       start=(j == 0), stop=(j == 3))
                    return ins
                P.add("pe", mm0, reads=rk(s0) + ukeys, writes=[("bank", b0)])
                P.add("pe", mm1, reads=rk(s1) + ukeys, writes=[("bank", b1)])
                P.add("pe", mmyp, reads=rk(sb) + [("zT", g, tc) for g in range(4)], writes=[("bank", byp)])
                P.add("pe", mmya, reads=rk(sb) + [("OT", j, e_, tc) for j in range(4) for e_ in range(2)],
                      writes=[("bank", bya)])
                ta_, tb_ = 2 * (tc % 2), 2 * (tc % 2) + 1
                P.add("act", lambda e, b0=b0, ta_=ta_: e.activation(tmp[:, ta_, :], bk(b0, 0, 512), AF.Sigmoid),
                      reads=[("bank", b0)], writes=[("tmp", ta_)])
                P.add("act", lambda e, b1=b1, tb_=tb_: e.activation(tmp[:, tb_, :], bk(b1, 0, 512), AF.Sigmoid),
                      reads=[("bank", b1)], writes=[("tmp", tb_)])
                P.add("dve", lambda e, byp=byp, ta_=ta_: e.tensor_tensor(tmp[:, ta_, :], tmp[:, ta_, :], bk(byp, 0, 512), ALU.mult),
                      reads=[("tmp", ta_), ("bank", byp)], writes=[("tmp", ta_)])
                P.add("dve", lambda e, bya=bya, tb_=tb_: e.tensor_tensor(tmp[:, tb_, :], tmp[:, tb_, :], bk(bya, 0, 512), ALU.mult),
                      reads=[("tmp", tb_), ("bank", bya)], writes=[("tmp", tb_)])
                P.add("dve", lambda e, ta_=ta_, tb_=tb_, c=c, tc=tc: e.tensor_tensor(
                    xb(MRG + c * 2048 + tc * 512, [[1, 512]]), tmp[:, ta_, :], tmp[:, tb_, :], ALU.add),
                    reads=[("tmp", ta_), ("tmp", tb_)], writes=[("mrg", c, tc)])
        if debug:
            P.dma("pool", lambda e: e.dma_start(out=dbg_mrg, in_=xb(MRG, [[2048, 8], [1, 2048]])),
                  reads=[("mrg", c, tc) for c in range(8) for tc in range(NTC)], sem_key="dbg_mrg", is_out=True)
        wslots = [ring_use("wout", c, la=7 - c) for c in range(8)]
        for t in range(NT):
            for hf in range(2):
                bi = nbank("all")

                def mmo(e, t=t, hf=hf, bi=bi):
                    ins = None
                    for c in range(8):
                        ins = e.matmul(bk(bi, 0, 512), xb(MRG + c * 2048 + t * 128, [[1, 128]]),
                                       bass.AP(ring, wslots[c] * 1024 + hf * 512, [[8192, 128], [1, 512]]),
                                       start=(c == 0), stop=(c == 7))
                    return ins
                P.add("pe", mmo, reads=[("mrg", c, t // 4) for c in range(8)] + [k for c in range(8) for k in rk(wslots[c])],
                      writes=[("bank", bi)])
                P.add("dve", lambda e, t=t, hf=hf, bi=bi: e.tensor_tensor(
                    h[:, t, hf * 512:(hf + 1) * 512], bk(bi, 0, 512), h[:, t, hf * 512:(hf + 1) * 512], ALU.add),
                    reads=[("bank", bi), ("h", t, hf)], writes=[("h", t, hf)])
        ring_emit_upto(ring_state["used"] + LOOKAHEAD)

    def final_norm(s, next_s=None):
        fence()
        norm_stats(3)
        for t in range(NT):
            b = t % 8
            P.add("dve", lambda e, t=t, b=b: e.scalar_tensor_tensor(
                xf(OBUF_F + b * 1024, [[1, 1024]]), h[:, t, :], rstd[:, t:t + 1], gain[:], ALU.mult, ALU.mult),
                reads=[("h", t, 0), ("h", t, 1), "rstd", "gain"], writes=[("obuf", b)])
            P.dma("sp", lambda e, t=t, b=b: e.dma_start(out=out_d[s, t * 128:(t + 1) * 128, :],
                                                         in_=xf(OBUF_F + b * 1024, [[1, 1024]])),
                  reads=[("obuf", b)], sem_key=("out", b), is_out=True)
            if next_s is not None:
                load_x_tile(next_s, t)

    def dump(i):
        if not debug:
            return
        P.dma("sp", lambda e, i=i: e.dma_start(out=dbg_d[i].rearrange("(t p) d -> p t d", p=128), in_=h[:]),
              reads=[("h", t, hf) for t in range(NT) for hf in range(2)], sem_key=("dbg", i), is_out=True)

    for s in range(nseq):
        if s == 0:
            load_x(s)
        ffn("w1g", "w1u", w1d_d, norm_to_uT(0))
        dump(0)
        attention(norm_to_uT(1))
        if debug:
            P.dma("pool", lambda e: e.dma_start(out=dbg_ot, in_=xb(OTO, [[2048, 4], [1, 2048]])),
                  reads=[("OT", j, e_, qc) for j in range(4) for e_ in range(2) for qc in range(NTC)], sem_key="dbg_ot", is_out=True)
        pool_mixer()
        if debug:
            P.dma("pool", lambda e: e.dma_start(out=dbg_zt, in_=xb(ZTO, [[2048, 4], [1, 2048]])),
                  reads=[("zT", g, tc) for g in range(4) for tc in range(NTC)], sem_key="dbg_zt", is_out=True)
        merge_and_out()
        dump(1)
        ffn("w2g", "w2u", w2d_d, norm_to_uT(2))
        dump(2)
        final_norm(s, s + 1 if s + 1 < nseq else None)

    P.resolve()
    with contextlib.ExitStack() as st:
        sems = {}
        for i, sn in enumerate(P.sem_names()):
            sems[sn] = st.enter_context(nc.semaphore(f"s{i}"))
        P.emit(nc, sems)
    return nc


def _tile_cols(w):
    K, N = w.shape
    return np.ascontiguousarray(w.reshape(K // 128, 128, N // 128, 128).transpose(2, 1, 0, 3))


def _consts():
    f = np.float32
    ident = np.eye(128, dtype=f)
    i = np.arange(128)
    tri = np.where(i[:, None] > i[None, :], f(MBIG), f(0.0)).astype(f)
    t = np.arange(S)
    slopes = np.exp2(-np.arange(1, 9, dtype=np.float64)).astype(f)
    alibiq = np.zeros((8, 2, S), f)
    for hh in range(8):
        alibiq[hh, 0] = -8.0 * slopes[hh] * ((t // 64) * 64)
        alibiq[hh, 1] = -8.0 * slopes[hh] * (t % 64)
    kaaug = np.zeros((10, S), f)
    for n in range(8):
        kaaug[n] = (t // 256 == n)
    kaaug[8:] = 1.0
    expb = np.zeros((128, 8, 16), f)
    for hh in range(8):
        for kt in range(16):
            expb[:, hh, kt] = slopes[hh] * (kt * 128 + i)
    invc = np.zeros((128, 4, 16), f)
    for g in range(4):
        w = 2 ** (g + 1)
        invc[:, g, :] = 1.0 / np.minimum(np.arange(16) + 1, w)
    tri2 = np.concatenate([np.full((128, 128), MBIG, f), tri], axis=1)
    return dict(ident=ident, tri=tri, tri2=tri2, alibiq=alibiq, alibik=-alibiq, kaaug=kaaug, expb=expb, invc=invc)


_NC_CACHE = {}


def kernel(x, ffn1_norm, ffn1_w_gate, ffn1_w_up, ffn1_w_down, mix_norm, w_in,
           pool_w, pool_scale, w_branch_pool, w_branch_attn, w_out,
           ffn2_norm, ffn2_w_gate, ffn2_w_up, ffn2_w_down, final_norm):
    f = np.float32
    x = np.asarray(x, f)
    A = lambda a: np.asarray(a, f)
    shared = dict(
        w1g=_tile_cols(A(ffn1_w_gate)[0]), w1u=_tile_cols(A(ffn1_w_up)[0]),
        w2g=_tile_cols(A(ffn2_w_gate)[0]), w2u=_tile_cols(A(ffn2_w_up)[0]),
        win=_tile_cols(A(w_in)[0]),
        wbp=_tile_cols(A(w_branch_pool)[0]), wba=_tile_cols(A(w_branch_attn)[0]),
        w1d=np.ascontiguousarray(A(ffn1_w_down)[0]), w2d=np.ascontiguousarray(A(ffn2_w_down)[0]),
        wout=np.ascontiguousarray(A(w_out)[0]),
        poolw=np.ascontiguousarray(A(pool_w)[0].transpose(1, 0, 2)),
        gains=np.ascontiguousarray(np.broadcast_to(
            np.stack([A(ffn1_norm)[0], A(mix_norm)[0], A(ffn2_norm)[0], A(final_norm)])[:, None, :], (4, 128, D))),
        pscale=np.ascontiguousarray(A(pool_scale)[0].reshape(4, 128).T),
    )
    shared.update(_consts())
    if "nc" not in _NC_CACHE:
        _NC_CACHE["nc"] = build_program(NSEQ)
    nc = _NC_CACHE["nc"]
    in_maps = []
    for c in range(NCORES):
        m = dict(shared)
        m["x"] = np.ascontiguousarray(x[c * NSEQ:(c + 1) * NSEQ])
        in_maps.append(m)
    res = run_bass_kernel_spmd(nc, in_maps, core_ids=list(range(NCORES)))
    return np.concatenate([np.asarray(r["out"], f) for r in res.results], axis=0)
```
